# Optimizing a Trainium2 kernel written in Bass

```python
import jax, jax.numpy as jnp
from jax import lax
import numpy as np

D_MODEL = 1024
BATCH = 8
SEQ = 4096
DEPTH = 2

D_MIX = 512
N_BRANCH = 3
GLA_HEADS = 4
GLA_DK = 64
GLA_DV = 128
GLA_RANK = 16
GLA_TAU = 16.0
GLA_CHUNK = 64
POOL_WINDOWS = (2, 4, 8, 16)
POOL_GROUPS = 4
POOL_GC = D_MIX // POOL_GROUPS
MOBA_HEADS = 8
MOBA_DH = D_MIX // MOBA_HEADS
MOBA_BLOCK = 256
MOBA_TOPK = 3
MOBA_QBLOCK = 128
D_FF = -(-8 * D_MODEL // (3 * 256)) * 256
EPS = 1e-6
NEG = -1e30

IN_SIZES = (GLA_HEADS * GLA_DK, GLA_HEADS * GLA_DK, GLA_HEADS * GLA_DV, GLA_RANK,
            GLA_HEADS * GLA_DV, D_MIX, D_MIX, D_MIX, D_MIX, N_BRANCH * D_MODEL)
D_IN = sum(IN_SIZES)

kernel_name = "hybrid_gla_pool_moba_block"


def rmsnorm(x, w):
    xf = x.astype(jnp.float32)
    y = xf * lax.rsqrt(jnp.mean(xf * xf, axis=-1, keepdims=True) + EPS)
    return (y * w.astype(jnp.float32)).astype(x.dtype)


def gla_mixer(q, k, v, g1, r, w_g2, b_g, norm_w):
    B, T, _ = q.shape
    H, DK, DV, C = GLA_HEADS, GLA_DK, GLA_DV, GLA_CHUNK
    NC = T // C
    f32 = jnp.float32
    qf = q.astype(f32).reshape(B, T, H, DK) * (DK ** -0.5)
    kf = k.astype(f32).reshape(B, T, H, DK)
    vf = v.astype(f32).reshape(B, T, H, DV)
    logit = (g1 @ w_g2 + b_g).astype(f32)
    log_a = (jax.nn.log_sigmoid(logit) / GLA_TAU).reshape(B, T, H, DK)

    def to_chunks(a):
        return a.reshape(B, NC, C, H, a.shape[-1]).transpose(1, 0, 3, 2, 4)

    causal = jnp.tril(jnp.ones((C, C), dtype=bool))

    def step(S, inp):
        qc, kc, vc, gc = inp
        bc = jnp.cumsum(gc, axis=2)
        diff = bc[:, :, :, None, :] - bc[:, :, None, :, :]
        decay = jnp.exp(jnp.where(causal[None, None, :, :, None], diff, NEG))
        attn = jnp.einsum('bhtd,bhsd,bhtsd->bhts', qc, kc, decay)
        o = (jnp.einsum('bhts,bhsv->bhtv', attn, vc)
             + jnp.einsum('bhtd,bhdv->bhtv', qc * jnp.exp(bc), S))
        b_last = bc[:, :, -1:, :]
        S = (jnp.exp(b_last[:, :, 0, :])[..., None] * S
             + jnp.einsum('bhsd,bhsv->bhdv', kc * jnp.exp(b_last - bc), vc))
        return S, o

    S0 = jnp.zeros((B, H, DK, DV), f32)
    _, o = lax.scan(step, S0, (to_chunks(qf), to_chunks(kf), to_chunks(vf), to_chunks(log_a)))
    o = o.transpose(1, 0, 3, 2, 4).reshape(B, T, H, DV)
    o = o * lax.rsqrt(jnp.mean(o * o, axis=-1, keepdims=True) + EPS) * norm_w.astype(f32)
    o = o.reshape(B, T, H * DV) * jax.nn.silu(r.astype(f32))
    return o.astype(q.dtype)


def pool_mixer(u, w_pool, scale):
    B, T, _ = u.shape
    f32 = jnp.float32
    uf = u.astype(f32).reshape(B, T, POOL_GROUPS, POOL_GC)
    cs = jnp.cumsum(uf, axis=1)
    t = jnp.arange(T)
    outs = []
    for gi, w in enumerate(POOL_WINDOWS):
        c = cs[:, :, gi]
        lag = jnp.pad(c, ((0, 0), (w, 0), (0, 0)))[:, :T]
        cnt = jnp.minimum(t + 1, w).astype(f32)[None, :, None]
        outs.append((c - lag) / cnt - uf[:, :, gi])
    p = jnp.stack(outs, axis=2)
    y = jnp.einsum('btgc,gcd->btgd', p, w_pool.astype(f32)).reshape(B, T, D_MIX)
    return (y * scale.astype(f32)).astype(u.dtype)


def moba_mixer(q, k, v):
    B, T, _ = q.shape
    H, DH, BLK, QB = MOBA_HEADS, MOBA_DH, MOBA_BLOCK, MOBA_QBLOCK
    NB = -(-T // BLK)
    NQ = T // QB
    KSEL = min(MOBA_TOPK, NB)
    f32 = jnp.float32
    qh = q.reshape(B, T, H, DH).transpose(0, 2, 1, 3)
    kh = k.reshape(B, T, H, DH).transpose(0, 2, 1, 3)
    vh = v.reshape(B, T, H, DH).transpose(0, 2, 1, 3)
    pad = NB * BLK - T
    kp = jnp.pad(kh, ((0, 0), (0, 0), (0, pad), (0, 0)))
    vp = jnp.pad(vh, ((0, 0), (0, 0), (0, pad), (0, 0)))
    kblk = kp.reshape(B, H, NB, BLK, DH)
    vblk = vp.reshape(B, H, NB, BLK, DH)
    kmean = jnp.mean(kblk.astype(f32), axis=3)
    gate = jnp.einsum('bhtd,bhnd->bhtn', qh.astype(f32), kmean)
    tblk = jnp.arange(T) // BLK
    past = jnp.arange(NB)[None, :] < tblk[:, None]
    gate = jnp.where(past[None, None], gate, NEG)
    _, idx = lax.top_k(gate, KSEL)
    valid = jnp.arange(KSEL)[None, :] < tblk[:, None]
    slopes = (2.0 ** (-8.0 * (jnp.arange(H) + 1) / H)).astype(f32)
    scale = DH ** -0.5
    hi = jnp.arange(H)[:, None, None]

    def one_block(i):
        b = i // NQ
        t0 = (i % NQ) * QB
        qb = lax.dynamic_slice(qh, (b, 0, t0, 0), (1, H, QB, DH))[0]
        ib = lax.dynamic_slice(idx, (b, 0, t0, 0), (1, H, QB, KSEL))[0]
        vm = lax.dynamic_slice(valid, (t0, 0), (QB, KSEL))
        kg = kblk[b][hi, ib]
        vg = vblk[b][hi, ib]
        own0 = (t0 // BLK) * BLK
        ko = lax.dynamic_slice(kp, (b, 0, own0, 0), (1, H, BLK, DH))[0]
        vo = lax.dynamic_slice(vp, (b, 0, own0, 0), (1, H, BLK, DH))[0]
        tq = t0 + jnp.arange(QB)
        s_sel = ib[..., None] * BLK + jnp.arange(BLK)
        s_own = own0 + jnp.arange(BLK)
        sc_sel = (jnp.einsum('hqd,hqkjd->hqkj', qb, kg).astype(f32) * scale
                  - slopes[:, None, None, None] * (tq[None, :, None, None] - s_sel).astype(f32))
        sc_sel = jnp.where(vm[None, :, :, None], sc_sel, NEG)
        sc_own = (jnp.einsum('hqd,hjd->hqj', qb, ko).astype(f32) * scale
                  - slopes[:, None, None] * (tq[:, None] - s_own[None, :]).astype(f32)[None])
        sc_own = jnp.where((s_own[None, :] <= tq[:, None])[None], sc_own, NEG)
        sc = jnp.concatenate([sc_sel.reshape(H, QB, KSEL * BLK), sc_own], axis=-1)
        p = jax.nn.softmax(sc, axis=-1).astype(v.dtype)
        p_sel = p[..., :KSEL * BLK].reshape(H, QB, KSEL, BLK)
        p_own = p[..., KSEL * BLK:]
        return (jnp.einsum('hqkj,hqkjd->hqd', p_sel, vg)
                + jnp.einsum('hqj,hjd->hqd', p_own, vo))

    out = lax.map(one_block, jnp.arange(B * NQ))
    return out.reshape(B, NQ, H, QB, DH).transpose(0, 1, 3, 2, 4).reshape(B, T, H * DH)


def setup_inputs(seed: int = 0) -> dict:
    key = jax.random.key(seed)
    ks = jax.random.split(key, 20)
    f32 = jnp.float32

    def nrm(k, shape, fan_in):
        return jax.random.normal(k, shape, f32) * (fan_in ** -0.5)

    def gain(k, shape):
        return 1.0 + 0.05 * jax.random.normal(k, shape, f32)

    L = DEPTH
    return {
        "x": jax.random.normal(ks[0], (BATCH, SEQ, D_MODEL), f32),
        "norm_mix_pre": gain(ks[1], (L, D_MODEL)),
        "w_in": nrm(ks[2], (L, D_MODEL, D_IN), D_MODEL),
        "gla_w_g2": nrm(ks[3], (L, GLA_RANK, GLA_HEADS * GLA_DK), GLA_RANK),
        "gla_b_g": 0.1 * jax.random.normal(ks[4], (L, GLA_HEADS * GLA_DK), f32),
        "gla_norm": gain(ks[5], (L, GLA_DV)),
        "pool_w": nrm(ks[6], (L, POOL_GROUPS, POOL_GC, POOL_GC), POOL_GC),
        "pool_scale": gain(ks[7], (L, D_MIX)),
        "w_branch_a": nrm(ks[8], (L, D_MIX, D_MODEL), D_MIX),
        "w_branch_b": nrm(ks[9], (L, D_MIX, D_MODEL), D_MIX),
        "w_branch_c": nrm(ks[10], (L, D_MIX, D_MODEL), D_MIX),
        "w_out": nrm(ks[11], (L, D_MODEL, D_MODEL), D_MODEL),
        "norm_mix_post": gain(ks[12], (L, D_MODEL)),
        "norm_ffn_pre": gain(ks[13], (L, D_MODEL)),
        "ffn_w_gate": nrm(ks[14], (L, D_MODEL, D_FF), D_MODEL),
        "ffn_w_up": nrm(ks[15], (L, D_MODEL, D_FF), D_MODEL),
        "ffn_w_down": nrm(ks[16], (L, D_FF, D_MODEL), D_FF),
        "norm_ffn_post": gain(ks[17], (L, D_MODEL)),
    }


def reference(x, norm_mix_pre, w_in, gla_w_g2, gla_b_g, gla_norm, pool_w, pool_scale,
              w_branch_a, w_branch_b, w_branch_c, w_out, norm_mix_post, norm_ffn_pre,
              ffn_w_gate, ffn_w_up, ffn_w_down, norm_ffn_post):
    B, T, D = x.shape
    splits = [int(s) for s in np.cumsum(IN_SIZES)[:-1]]
    for l in range(DEPTH):
        h = rmsnorm(x, norm_mix_pre[l])
        proj = h @ w_in[l]
        gq, gk, gv, gg1, gr, pu, mq, mk, mv, gates = jnp.split(proj, splits, axis=-1)
        ya = gla_mixer(gq, gk, gv, gg1, gr, gla_w_g2[l], gla_b_g[l], gla_norm[l]) @ w_branch_a[l]
        yb = pool_mixer(pu, pool_w[l], pool_scale[l]) @ w_branch_b[l]
        yc = moba_mixer(mq, mk, mv) @ w_branch_c[l]
        gs = jax.nn.sigmoid(gates).reshape(B, T, N_BRANCH, D)
        mixed = gs[:, :, 0] * ya + gs[:, :, 1] * yb + gs[:, :, 2] * yc
        x = x + rmsnorm(mixed @ w_out[l], norm_mix_post[l])
        h = rmsnorm(x, norm_ffn_pre[l])
        f = (jax.nn.silu(h @ ffn_w_gate[l]) * (h @ ffn_w_up[l])) @ ffn_w_down[l]
        x = x + rmsnorm(f, norm_ffn_post[l])
    return x
```

```python
import contextlib
import numpy as np
import concourse.bass as bass
import concourse.mybir as mybir
from concourse.bass_utils import run_bass_kernel_spmd

F32 = mybir.dt.float32
BF16 = mybir.dt.bfloat16
AF = mybir.ActivationFunctionType
ALU = mybir.AluOpType
AX = mybir.AxisListType

D = 1024
DIN = 6672
DFF = 2816
EPS = 1e-6
NEGV = -30000.0
NEGBIG = -1.0e30
WIN = [2, 4, 8, 16, 999, 999, 999, 999]
SLOPES = [2.0 ** (-(h + 1)) for h in range(8)]
WBUF_EL = 4608
N_WBUF = 3
LOADQ = "pool"
SAME_ENG_SYNC = True


class DSem:
    __slots__ = ("key", "cnt")

    def __init__(self, key):
        self.key = key
        self.cnt = 0


class Res:
    __slots__ = ("name", "w", "r", "dsem", "excl")

    def __init__(self, name, excl=False):
        self.name = name
        self.w = None
        self.r = {}
        self.dsem = None
        self.excl = excl


class Sched:
    ENGS = ("pe", "act", "dve", "pool", "sp")

    def __init__(self, nc, es):
        self.nc = nc
        self.es = es
        self.q = {e: [] for e in self.ENGS}
        self.sems = {}
        for e in self.ENGS:
            self.sems[e] = es.enter_context(nc.semaphore("sem_" + e))
        self.cnt = {e: 0 for e in self.ENGS}
        self.seen = {e: {} for e in self.ENGS}
        self.ndsem = 0

    def new_dsem(self):
        k = "dma%d" % self.ndsem
        self.ndsem += 1
        self.sems[k] = self.es.enter_context(self.nc.semaphore("sem_" + k))
        return DSem(k)

    def _waits(self, eng, reads, writes):
        seen = self.seen[eng]
        best = {}

        def add(k, v):
            if k == eng and (eng == "pe" or not SAME_ENG_SYNC):
                return
            if seen.get(k, 0) >= v:
                return
            if best.get(k, 0) < v:
                best[k] = v
        for r in reads:
            if r.w is not None:
                add(*r.w)
            if r.excl:
                for k, v in r.r.items():
                    if k != eng:
                        add(k, v)
        for w in writes:
            if w.w is not None:
                add(*w.w)
            for k, v in w.r.items():
                add(k, v)
        out = []
        for k, v in best.items():
            seen[k] = v
            out.append((k, v))
        return out

    def op(self, eng, fn, reads=(), writes=()):
        waits = self._waits(eng, reads, writes)
        self.cnt[eng] += 1
        tokv = self.cnt[eng]
        for r in reads:
            if r.r.get(eng, 0) < tokv:
                r.r[eng] = tokv
        for w in writes:
            w.w = (eng, tokv)
            w.r = {}
        self.q[eng].append((waits, fn, eng, 1))

    def dma(self, qeng, out, in_, reads=(), writes=(), nowait=False):
        waits = [] if nowait else self._waits(qeng, reads, writes)
        tgt = writes[0]
        if tgt.dsem is None:
            tgt.dsem = self.new_dsem()
        ds = tgt.dsem
        ds.cnt += 16
        tok = (ds.key, ds.cnt)
        for r in reads:
            if r.r.get(tok[0], 0) < tok[1]:
                r.r[tok[0]] = tok[1]
        for w in writes:
            w.w = tok
            w.r = {}

        def fn(e, out=out, in_=in_):
            return e.dma_start(out=out, in_=in_)
        self.q[qeng].append((waits, fn, ds.key, 16))

    def final_wait(self, eng, ress):
        waits = self._waits(eng, ress, ())
        self.q[eng].append((waits, None, None, 0))

    def replay(self):
        nc = self.nc
        sems = self.sems
        q = self.q
        with nc.Block() as block:
            def run(e, name):
                for waits, fn, isem, iv in q[name]:
                    for k, v in waits:
                        e.wait_ge(sems[k], v)
                    if fn is None:
                        continue
                    ins = fn(e)
                    if isem is not None:
                        ins.then_inc(sems[isem], iv)

            @block.tensor
            def _(e):
                run(e, "pe")

            @block.scalar
            def _(e):
                run(e, "act")

            @block.vector
            def _(e):
                run(e, "dve")

            @block.gpsimd
            def _(e):
                run(e, "pool")

            @block.sync
            def _(e):
                run(e, "sp")


def v3(ap, a):
    return ap.rearrange("p (a b) -> p a b", a=a)


class Prog:
    def __init__(self, T, nlayers, dbg=False):
        self.T = T
        self.NL = nlayers
        self.NT = T // 128
        self.NG = T // 512
        self.dbg = dbg
        self.nc = bass.Bass("TRN2", target_bir_lowering=False)

    def sb(self, name, shape, dt):
        return self.es.enter_context(self.nc.sbuf_tensor(name, shape, dt))

    def mm(self, out, lhsT, rhs, start, stop, reads, writes, skip=False):
        if skip:
            self.S.op("pe", lambda e, o=out, l=lhsT, r=rhs, s=start, p=stop: e.matmul(o, lhsT=l, rhs=r, start=s, stop=p,
                                                                                     skip_group_check=True), reads, writes)
        else:
            self.S.op("pe", lambda e, o=out, l=lhsT, r=rhs, s=start, p=stop: e.matmul(o, lhsT=l, rhs=r, start=s, stop=p),
                      reads, writes)

    def tr(self, out, in_, reads, writes):
        idn = self.identB[:, :]
        self.S.op("pe", lambda e, o=out, i=in_, d=idn: e.transpose(o, i, d), list(reads) + [self.r_const], writes)

    def act(self, out, in_, func, reads, writes, bias=None, scale=None, accum_out=None):
        kw = {}
        if bias is not None:
            kw["bias"] = bias
        if scale is not None:
            kw["scale"] = scale
        if accum_out is not None:
            kw["accum_out"] = accum_out
        self.S.op("act", lambda e, o=out, i=in_, f=func, kw=kw: e.activation(out=o, in_=i, func=f, **kw), reads, writes)

    def tt(self, out, in0, in1, op, reads, writes, eng="dve"):
        self.S.op(eng, lambda e, o=out, a=in0, b=in1, op=op: e.tensor_tensor(out=o, in0=a, in1=b, op=op), reads, writes)

    def ts(self, out, in0, s1, op0, reads, writes, s2=None, op1=None, eng="dve"):
        if op1 is None:
            self.S.op(eng, lambda e, o=out, a=in0, s1=s1, op0=op0: e.tensor_scalar(out=o, in0=a, scalar1=s1, scalar2=0.0, op0=op0,
                                                                                  op1=ALU.add), reads, writes)
        else:
            self.S.op(eng, lambda e, o=out, a=in0, s1=s1, s2=s2, op0=op0, op1=op1:
                      e.tensor_scalar(out=o, in0=a, scalar1=s1, scalar2=s2, op0=op0, op1=op1), reads, writes)

    def stt(self, out, in0, scalar, in1, op0, op1, reads, writes, eng="dve"):
        self.S.op(eng, lambda e, o=out, a=in0, s=scalar, b=in1, op0=op0, op1=op1:
                  e.scalar_tensor_tensor(out=o, in0=a, scalar=s, in1=b, op0=op0, op1=op1), reads, writes)

    def red(self, out, in_, op, reads, writes):
        self.S.op("dve", lambda e, o=out, i=in_, op=op: e.tensor_reduce(out=o, in_=i, axis=AX.X, op=op), reads, writes)

    def cp(self, eng, out, in_, reads, writes):
        if eng == "act":
            self.S.op("act", lambda e, o=out, i=in_: e.copy(out=o, in_=i), reads, writes)
        else:
            self.S.op(eng, lambda e, o=out, i=in_: e.tensor_copy(out=o, in_=i), reads, writes)

    def memset(self, eng, ap, val, writes):
        self.S.op(eng, lambda e, a=ap, v=val: e.memset(a, v), (), writes)

    def nb(self):
        i = self.rot_set[self.rot_i % len(self.rot_set)]
        self.rot_i += 1
        return self.bank[i], self.r_bank[i]

    def bankb(self, bank):
        return bank[:, :].bitcast(BF16)

    def t2k(self):
        i = self.t2k_i % len(self.T2K)
        self.t2k_i += 1
        return self.T2K[i], self.r_T2K[i]

    def t1k(self):
        i = self.t1k_i % len(self.T1K)
        self.t1k_i += 1
        return self.T1K[i], self.r_T1K[i]

    def small(self):
        i = self.sm_i % len(self.SM)
        self.sm_i += 1
        return self.SM[i], self.r_SM[i]

    def block_seq(self):
        seq = []
        for li in range(self.NL):
            for g in range(self.NG):
                seq += [(li, "B%d" % b) for b in range(7)]
                seq += [(li, "M%d" % c) for c in range(8)]
                seq += [(li, "O0"), (li, "O1")]
                seq += [(li, "GU%d" % j) for j in range(11)]
                seq += [(li, "DN%d" % j) for j in range(11)] * 2
        return seq

    def prefetch(self):
        if self.wpos_load >= len(self.wseq):
            return
        i = self.wpos_load % N_WBUF
        ap, res, nel = self.blocks[self.wseq[self.wpos_load]]
        self.wpos_load += 1
        self.S.dma(LOADQ, self.wbuf[i][:, 0:nel], ap[:, 0:nel], reads=[res], writes=[self.r_wbuf[i]])

    def load_block(self, li, name):
        assert self.wseq[self.wpos_use] == (li, name), (self.wseq[self.wpos_use], li, name)
        while self.wpos_load <= self.wpos_use:
            self.prefetch()
        i = self.wpos_use % N_WBUF
        self.wpos_use += 1
        return self.wbuf[i], self.r_wbuf[i]

    def done_block(self):
        self.prefetch()

    def build(self):
        nc = self.nc
        T, NL, NT, NG = self.T, self.NL, self.NT, self.NG
        with contextlib.ExitStack() as es:
            self.es = es
            self.S = Sched(nc, es)
            S = self.S

            def din(name, shape):
                return nc.dram_tensor(name, shape, F32, kind="ExternalInput").ap()
            self.x_in = din("x", [T, D])
            self.w_in = din("w_in", [NL, D, DIN])
            self.w_g2 = din("gla_w_g2", [NL, 16, 256])
            self.pool_w = din("pool_w", [NL, 4, 128, 128])
            self.w_br = [din("w_branch_" + c, [NL, 512, D]) for c in "abc"]
            self.w_out = din("w_out", [NL, D, D])
            self.w_gate = din("ffn_w_gate", [NL, D, DFF])
            self.w_up = din("ffn_w_up", [NL, D, DFF])
            self.w_down = din("ffn_w_down", [NL, DFF, D])
            self.vecf_d = din("vecf", [NL, 128, 20])
            self.rowbc_d = din("rowbc", [NL, 128, 2432])
            c_ident = din("c_ident", [128, 128])
            c_tri = din("c_tri", [128, 128])
            c_negtri = din("c_negtri", [128, 128])
            c_pool = din("c_pool", [12, 128, 128])
            c_e16 = din("c_e16", [128, 16 * 128])
            c_alibi = din("c_alibi", [128, 8 * 33])
            self.y = nc.dram_tensor("y", [T, D], F32, kind="ExternalOutput").ap()
            self.xres = nc.dram_tensor("xres", [T, D], F32).ap() if NL > 1 else None
            if self.dbg:
                self.dbg_selb = nc.dram_tensor("dbg_selb", [NG, 128, 4, 128], BF16, kind="ExternalOutput").ap()
                self.r_dbg = Res("dbg")
                self.dbg_C = nc.dram_tensor("dbg_C", [NG, 128, 4, 512], BF16, kind="ExternalOutput").ap()
                self.r_dbg2 = Res("dbg2")
                self.dbg_selT = nc.dram_tensor("dbg_selT", [16, 512], BF16, kind="ExternalOutput").ap()
                self.r_dbg3 = Res("dbg3")
                self.dbg_PT = nc.dram_tensor("dbg_PT", [128, 512], BF16, kind="ExternalOutput").ap()
                self.r_dbg4 = Res("dbg4")
            self.r_xrow = [[Res("xrow%d_%d" % (l, t)) for t in range(NT)] for l in range(NL + 1)]
            st_sems = [S.new_dsem() for _ in range(4)]
            for l in range(1, NL + 1):
                for t in range(NT):
                    self.r_xrow[l][t].dsem = st_sems[t % 4]

            sb = self.sb
            self.identB = sb("identB", [128, 128], BF16)
            self.triF = sb("triF", [128, 128], F32)
            self.triB = sb("triB", [128, 128], BF16)
            self.negtri = sb("negtri", [128, 128], BF16)
            self.cpool = sb("cpool", [128, 12, 128], BF16)
            self.e16 = sb("e16", [128, 16, 128], BF16)
            self.alibi = sb("alibi", [128, 8 * 33], F32)
            self.cst = sb("cst", [128, 4], F32)
            self.r_const = Res("const")
            self.r_constf = Res("constf")
            self.vecf = sb("vecf_s", [128, 20], F32)
            self.rowbc = sb("rowbc_s", [128, 2432], F32)
            self.wg2 = sb("wg2", [16, 256], BF16)
            self.wpool = sb("wpool", [128, 4, 128], BF16)
            self.r_layer = Res("layerconst")
            self.r_layerb = Res("layerconstb")
            self.kTm = sb("kTm", [128, 4, T], BF16)
            self.r_kTm = [Res("kTm%d" % g) for g in range(NG)]
            self.Vaug = sb("Vaug", [128, NT, 8, 65], BF16)
            self.r_V = [Res("V%d" % t) for t in range(NT)]
            self.kmBD = sb("kmBD", [128, 4, 32], BF16)
            self.kmF = sb("kmF", [128, 4, 32], F32)
            self.r_kmBD = Res("kmBD")
            self.ksum = sb("ksum", [128, 8], F32)
            self.r_ksum = Res("ksum")
            self.Sf = sb("Sf", [128, 256], F32)
            self.Sb = sb("Sb", [128, 256], BF16)
            self.r_Sf = Res("Sf")
            self.r_Sb = Res("Sb")
            self.xg = sb("xg", [128, 4, D], F32)
            self.r_xg = [Res("xg%d" % t) for t in range(4)]
            self.hT = sb("hT", [128, 8, 512], BF16)
            self.r_hT = Res("hT")
            self.xs = sb("xs", [128, D], BF16)
            self.r_xs = Res("xs")
            self.wbuf = [sb("wbuf%d" % i, [128, WBUF_EL], BF16) for i in range(N_WBUF)]
            self.r_wbuf = [Res("wbuf%d" % i) for i in range(N_WBUF)]
            self.qkg = sb("qkg", [128, 4, 512], F32)
            self.r_qkg = Res("qkg")
            self.g1T = sb("g1T", [16, 512], BF16)
            self.r_g1T = Res("g1T")
            self.vc = sb("vc", [128, 4, 512], BF16)
            self.r_vc = [Res("vc%d" % t) for t in range(4)]
            self.rq = sb("rq", [128, 4, 512], BF16)
            self.r_rq = Res("rq")
            self.u = sb("u", [128, 5, 512], BF16)
            self.r_u = [Res("u%d" % t) for t in range(5)]
            self.selb = sb("selb", [128, 4, 128], BF16)
            self.selTb = [sb("selT%d" % i, [128, 512], BF16) for i in range(2)]
            self.r_selTb = [Res("selT%d" % i) for i in range(2)]
            self.r_selb = Res("selb")
            self.big = sb("big", [128, 22, 512], BF16)
            self.r_big = [Res("big%d" % c) for c in range(22)]
            self.T2K = [sb("t2k%d" % i, [128, 512], F32) for i in range(5)]
            self.r_T2K = [Res("t2k%d" % i) for i in range(5)]
            self.T1K = [sb("t1k%d" % i, [128, 512], BF16) for i in range(6)]
            self.r_T1K = [Res("t1k%d" % i) for i in range(6)]
            self.SM = [sb("sm%d" % i, [128, 8], F32) for i in range(8)]
            self.r_SM = [Res("sm%d" % i) for i in range(8)]
            self.bank = [es.enter_context(nc.psum_tensor("bank%d" % i, [128, 512], F32)) for i in range(8)]
            self.r_bank = [Res("bank%d" % i, excl=True) for i in range(8)]
            self.rot_set = [0, 1, 2, 3, 4, 5]
            self.rot_i = self.t2k_i = self.t1k_i = self.sm_i = self.wb_i = 0
            print("sbuf bytes remaining after alloc", nc.sbuf_bytes_remaining)

            rc = self.r_const
            S.dma("pool", self.identB[:, :], c_ident[:, :], writes=[rc])
            S.dma("sp", self.triF[:, :], c_tri[:, :], writes=[self.r_constf])
            S.dma("pool", self.triB[:, :], c_tri[:, :], writes=[rc])
            S.dma("pool", self.negtri[:, :], c_negtri[:, :], writes=[rc])
            S.dma("pool", self.cpool[:, :, :], c_pool.rearrange("m s t -> s m t"), writes=[rc])
            S.dma("pool", self.e16[:, :, :], c_e16.rearrange("p (n s) -> p n s", n=16), writes=[rc])
            S.dma("sp", self.alibi[:, :], c_alibi[:, :], writes=[self.r_constf])
            r_cst = Res("cst")
            self.r_cst = r_cst
            self.memset("dve", self.cst[:, 0:1], 1.0, [r_cst])
            self.memset("dve", self.cst[:, 1:2], EPS, [r_cst])
            self.memset("dve", self.cst[:, 2:3], 0.0, [r_cst])
            self.memset("dve", self.Vaug[:, :, :, 64:65], 1.0, self.r_V)

            self.blocks = {}
            self.stage_res = {}
            for li in range(NL):
                self.cast_layer(li)

            self.wseq = self.block_seq()
            self.wpos_load = self.wpos_use = 0
            for _ in range(N_WBUF):
                self.prefetch()
            for li in range(NL):
                self.layer(li)

            outs = list(self.r_xrow[NL])
            if self.dbg:
                outs.append(self.r_dbg)
                outs.append(self.r_dbg2)
                outs.append(self.r_dbg3)
                outs.append(self.r_dbg4)
            S.final_wait("sp", outs)
            S.replay()
        return nc

    def new_block(self, li, name, nel):
        ap = self.nc.dram_tensor("ws_%d_%s" % (li, name), [128, nel], BF16).ap()
        stage = 0 if name[0] == "B" else (1 if name[0] in "MO" else 2)
        key = (li, stage)
        if key not in self.stage_res:
            self.stage_res[key] = Res("ws_%d_s%d" % key)
        res = self.stage_res[key]
        self.blocks[(li, name)] = (ap, res, nel)
        return ap, res

    def cast_layer(self, li):
        S = self.S
        wv = self.w_in[li].rearrange("(k p) n -> p k n", p=128)
        ap, res = self.new_block(li, "B0", 4224)
        d = v3(ap[:, 0:4224], 8)
        S.dma("pool", d[:, :, 0:512], wv[:, :, 0:512], writes=[res], nowait=True)
        S.dma("pool", d[:, :, 512:528], wv[:, :, 1024:1040], writes=[res], nowait=True)
        for bi, c0 in ((1, 512), (2, 1040), (3, 1552), (4, 2064), (5, 2576), (6, 3088)):
            ap, res = self.new_block(li, "B%d" % bi, 4096)
            S.dma("pool", v3(ap[:, 0:4096], 8), wv[:, :, c0:c0 + 512], writes=[res], nowait=True)
        for c in range(8):
            ap, res = self.new_block(li, "M%d" % c, 4608)
            dg = ap[:, 0:3072].rearrange("p (k i c) -> p k i c", k=8, i=3)
            db = ap[:, 3072:4608].rearrange("p (i k c) -> p i k c", i=3, k=4)
            for i in range(3):
                c0 = 3600 + i * 1024 + c * 128
                S.dma("pool", dg[:, :, i, :], wv[:, :, c0:c0 + 128], writes=[res], nowait=True)
                bv = self.w_br[i][li].rearrange("(k p) n -> p k n", p=128)
                S.dma("pool", db[:, i, :, :], bv[:, :, c * 128:(c + 1) * 128], writes=[res], nowait=True)
        ov = self.w_out[li].rearrange("(k p) n -> p k n", p=128)
        for hf in range(2):
            ap, res = self.new_block(li, "O%d" % hf, 4096)
            S.dma("pool", v3(ap[:, 0:4096], 8), ov[:, :, hf * 512:(hf + 1) * 512], writes=[res], nowait=True)
        gv = self.w_gate[li].rearrange("(k p) n -> p k n", p=128)
        uv = self.w_up[li].rearrange("(k p) n -> p k n", p=128)
        for j in range(11):
            ap, res = self.new_block(li, "GU%d" % j, 4096)
            dd = ap[:, 0:4096].rearrange("p (k g c) -> p k g c", k=8, g=2)
            S.dma("pool", dd[:, :, 0, :], gv[:, :, j * 256:(j + 1) * 256], writes=[res], nowait=True)
            S.dma("pool", dd[:, :, 1, :], uv[:, :, j * 256:(j + 1) * 256], writes=[res], nowait=True)
        for j in range(11):
            ap, res = self.new_block(li, "DN%d" % j, 2048)
            src = self.w_down[li][j * 256:(j + 1) * 256, :].rearrange("(m p) n -> p m n", p=128)
            S.dma("pool", v3(ap[:, 0:2048], 2), src, writes=[res], nowait=True)

    def layer(self, li):
        S = self.S
        rl = self.r_layer
        S.dma("sp", self.vecf[:, :], self.vecf_d[li], writes=[rl])
        S.dma("sp", self.rowbc[:, :], self.rowbc_d[li], writes=[rl])
        S.dma("pool", self.wg2[:, :], self.w_g2[li], writes=[self.r_layerb])
        S.dma("pool", self.wpool[:, :, :], self.pool_w[li].rearrange("g c d -> c g d"), writes=[self.r_layerb])
        self.memset("dve", self.kmBD[:, :, :], 0.0, [self.r_kmBD])
        self.memset("dve", self.kmF[:, :, :], 0.0, [self.r_kmBD])
        self.cur_li = li
        for g in range(self.NG):
            self.group(li, g)

    def xsrc(self, li):
        return self.x_in if li == 0 else self.xres

    def xdst(self, li):
        return self.y if li == self.NL - 1 else self.xres

    def norm_T(self, nwc):
        for t in range(4):
            xt = self.xg[:, t, :]
            rx = self.r_xg[t]
            st, rst = self.small()
            self.act(self.xs[:, :], xt, AF.Square, [rx], [self.r_xs, rst], accum_out=st[:, 0:1])
            self.act(st[:, 1:2], st[:, 0:1], AF.Ln, [rst, self.r_cst], [rst], bias=self.cst[:, 1:2], scale=1.0 / D)
            self.act(st[:, 1:2], st[:, 1:2], AF.Exp, [rst], [rst], scale=-0.5)
            self.act(self.xs[:, :], xt, AF.Copy, [rx, rst], [self.r_xs], scale=st[:, 1:2])
            for half in range(2):
                bank, rb = self.nb()
                bv = self.bankb(bank)
                for c in range(4):
                    cc = half * 4 + c
                    self.tr(bv[:, c * 128:(c + 1) * 128], self.xs[:, cc * 128:(cc + 1) * 128], [self.r_xs], [rb])
                self.tt(self.hT[:, half * 4:(half + 1) * 4, t * 128:(t + 1) * 128], v3(bv[:, 0:512], 4),
                        self.vecf[:, nwc + half * 4:nwc + half * 4 + 4].unsqueeze(2).to_broadcast([128, 4, 128]),
                        ALU.mult, [rb, self.r_layer], [self.r_hT])

    def proj_fm(self, W, m0, rw, evac):
        bank, rb = self.nb()
        for kc in range(8):
            self.mm(bank[:, :], W[:, kc, m0:m0 + 128], self.hT[:, kc, :], kc == 0, kc == 7, [rw, self.r_hT], [rb])
        evac(bank, rb)

    def proj_tm(self, W, rw, t, evac):
        bank, rb = self.nb()
        for kc in range(8):
            self.mm(bank[:, :], self.hT[:, kc, t * 128:(t + 1) * 128], W[:, kc, 0:512], kc == 0, kc == 7,
                    [rw, self.r_hT], [rb])
        evac(bank, rb)

    def post_norm_residual(self, mo, rmo, wrow, xt, rx):
        st, rst = self.small()
        self.act(self.xs[:, :], mo, AF.Square, [rmo], [self.r_xs, rst], accum_out=st[:, 0:1])
        self.act(st[:, 1:2], st[:, 0:1], AF.Ln, [rst, self.r_cst], [rst], bias=self.cst[:, 1:2], scale=1.0 / D)
        self.act(st[:, 1:2], st[:, 1:2], AF.Exp, [rst], [rst], scale=-0.5)
        self.stt(mo, mo, st[:, 1:2], wrow, ALU.mult, ALU.mult, [rmo, rst, self.r_layer], [rmo])
        self.tt(xt, xt, mo, ALU.add, [rx, rmo], [rx])

    def group(self, li, g):
        S = self.S
        T = self.T
        src = self.xsrc(li)
        dst = self.xdst(li)
        self.rot_set = [0, 1, 2, 3, 4, 5]
        for t in range(4):
            tt_ = g * 4 + t
            S.dma("sp", self.xg[:, t, :], src[tt_ * 128:(tt_ + 1) * 128, :], reads=[self.r_xrow[li][tt_]],
                  writes=[self.r_xg[t]])
        STOP = getattr(self, "stop", 99)
        if STOP >= 1:
            self.group_body(li, g, STOP)
        for t in range(4):
            tt_ = g * 4 + t
            S.dma("sp", dst[tt_ * 128:(tt_ + 1) * 128, :], self.xg[:, t, :], reads=[self.r_xg[t]],
                  writes=[self.r_xrow[li + 1][tt_]])

    def group_body(self, li, g, STOP):
        S = self.S
        self.norm_T(0)
        if STOP < 2:
            return

        wb, rw = self.load_block(li, "B0")
        W = v3(wb[:, 0:4224], 8)
        for m in range(4):
            self.proj_fm(W, m * 128, rw,
                         lambda bank, rb, m=m: self.cp("act", self.qkg[:, m, :], bank[:, :], [rb], [self.r_qkg]))
        if STOP < 2.2:
            return
        bank, rb = self.nb()
        for kc in range(8):
            self.mm(bank[0:16, :], W[:, kc, 512:528], self.hT[:, kc, :], kc == 0, kc == 7, [rw, self.r_hT], [rb])
        self.cp("act", self.g1T[:, :], bank[0:16, :], [rb], [self.r_g1T])
        self.done_block()
        if STOP < 2.4:
            return
        wb, rw = self.load_block(li, "B1")
        W = v3(wb[:, 0:4096], 8)
        for t in range(4):
            self.proj_tm(W, rw, t, lambda bank, rb, t=t: self.cp("act", self.vc[:, t, :], bank[:, :], [rb], [self.r_vc[t]]))
        self.done_block()
        if STOP < 2.6:
            return
        wb, rw = self.load_block(li, "B2")
        W = v3(wb[:, 0:4096], 8)
        for t in range(4):
            self.proj_tm(W, rw, t, lambda bank, rb, t=t: self.act(self.rq[:, t, :], bank[:, :], AF.Silu, [rb], [self.r_rq]))
        self.done_block()
        if STOP < 2.8:
            return
        wb, rw = self.load_block(li, "B3")
        W = v3(wb[:, 0:4096], 8)
        for t in range(4):
            self.proj_tm(W, rw, t, lambda bank, rb, t=t: self.cp("act", self.u[:, 1 + t, :], bank[:, :], [rb], [self.r_u[1 + t]]))
        self.done_block()

        if STOP < 3:
            return
        for t in range(4):
            self.gla_tile(g, t)
        if STOP < 3.99:
            return
        if STOP < 4:
            return
        for t in range(4):
            self.pool_tile(g, t)
        if STOP < 5:
            return
        self.cp("dve", self.u[:, 0, :], self.u[:, 4, :], [self.r_u[4]], [self.r_u[0]])

        wb, rw = self.load_block(li, "B4")
        W = v3(wb[:, 0:4096], 8)
        for m in range(4):
            def evq(bank, rb, m=m):
                self.cp("act", self.rq[:, m, :], bank[:, :], [rb], [self.r_rq])
            self.proj_fm(W, m * 128, rw, evq)
        self.done_block()
        wb, rw = self.load_block(li, "B5")
        W = v3(wb[:, 0:4096], 8)
        for m in range(4):
            def ev(bank, rb, m=m):
                self.cp("act", self.kTm[:, m, g * 512:(g + 1) * 512], bank[:, :], [rb], [self.r_kTm[g]])
                self.red(self.ksum[:, 2 * m:2 * m + 2], v3(bank[:, :], 2), ALU.add, [rb], [self.r_ksum])
            self.proj_fm(W, m * 128, rw, ev)
        self.done_block()
        ks = v3(self.ksum[:, 0:8], 4)
        for (r0, c0) in ((0, 2 * g), (64, 16 + 2 * g)):
            rows = slice(r0, r0 + 64)
            cs = slice(c0, c0 + 2)
            self.ts(self.kmF[rows, :, cs], ks[rows, :, :], 1.0 / 256, ALU.mult, [self.r_ksum], [self.r_kmBD])
            self.cp("dve", self.kmBD[rows, :, cs], self.kmF[rows, :, cs], [self.r_kmBD], [self.r_kmBD])
        wb, rw = self.load_block(li, "B6")
        W = v3(wb[:, 0:4096], 8)
        for t in range(4):
            tt_ = g * 4 + t
            self.proj_tm(W, rw, t, lambda bank, rb, tt_=tt_: self.cp("act", self.Vaug[:, tt_, :, 0:64], v3(bank[:, :], 8),
                                                                      [rb], [self.r_V[tt_]]))
        self.done_block()
        if STOP < 6:
            return
        self.moba_group(g)
        if STOP < 7:
            return
        self.mix_out(li, g)
        if STOP < 8:
            return
        self.ffn(li, g)

    def gla_tile(self, g, t):
        tt_ = g * 4 + t
        cols = slice(t * 128, (t + 1) * 128)
        rl = self.r_layer
        bank, rb = self.nb()
        self.mm(bank[:, 0:256], self.g1T[0:16, cols], self.wg2[0:16, :], True, True, [self.r_g1T, self.r_layerb], [rb])
        lg, rlg = self.t2k()
        self.tt(lg[:, 0:256], bank[:, 0:256], self.rowbc[:, 2048:2304], ALU.add, [rb, rl], [rlg])
        self.act(lg[:, 0:256], lg[:, 0:256], AF.Exp, [rlg], [rlg], scale=-1.0)
        la, rla = self.t2k()
        self.act(la[:, 0:256], lg[:, 0:256], AF.Ln, [rlg, self.r_cst], [rla], bias=self.cst[:, 0:1], scale=1.0)
        if getattr(self, "stop", 99) < 3.1:
            return
        lhi, rlhi = self.t1k()
        self.cp("dve", lhi[:, 0:256], la[:, 0:256], [rla], [rlhi])
        llo, rllo = self.t1k()
        self.tt(llo[:, 0:256], la[:, 0:256], lhi[:, 0:256], ALU.subtract, [rla, rlhi], [rllo])
        cum, rcum = self.nb()
        for p in range(2):
            self.mm(cum[:, p * 128:(p + 1) * 128], lhi[:, p * 128:(p + 1) * 128], self.triB[:, :], True, False,
                    [rlhi, self.r_const], [rcum])
            self.mm(cum[:, p * 128:(p + 1) * 128], llo[:, p * 128:(p + 1) * 128], self.triB[:, :], False, True,
                    [rllo, self.r_const], [rcum])
        if getattr(self, "stop", 99) < 3.12:
            return
        eq, req = self.t2k()
        self.act(eq[:, 0:256], cum[:, 0:256], AF.Exp, [rcum], [req], scale=-1.0 / 16)
        ek, rek = self.t2k()
        self.act(ek[:, 0:256], cum[:, 0:256], AF.Exp, [rcum], [rek], scale=1.0 / 16)
        if getattr(self, "stop", 99) < 3.13:
            return
        st, rst = self.small()
        for p in range(2):
            self.ts(st[:, p:p + 1], cum[:, p * 128 + 127:p * 128 + 128], -1.0 / 16, ALU.mult, [rcum], [rst])
        self.act(st[:, 2:4], st[:, 0:2], AF.Exp, [rst], [rst])
        ekk, rekk = self.t2k()
        for p in range(2):
            self.act(ekk[:, p * 128:(p + 1) * 128], cum[:, p * 128:(p + 1) * 128], AF.Exp, [rcum, rst], [rekk],
                     bias=st[:, p:p + 1], scale=1.0 / 16)
        if getattr(self, "stop", 99) < 3.2:
            return
        qp, rqp = self.t1k()
        self.stt(v3(qp[:, 0:256], 2), self.qkg[:, 0:2, cols], 0.125, v3(eq[:, 0:256], 2), ALU.mult, ALU.mult,
                 [self.r_qkg, req], [rqp])
        kp, rkp = self.t1k()
        self.tt(v3(kp[:, 0:256], 2), self.qkg[:, 2:4, cols], v3(ek[:, 0:256], 2), ALU.mult, [self.r_qkg, rek], [rkp])
        kpp, rkpp = self.t1k()
        self.tt(v3(kpp[:, 0:256], 2), self.qkg[:, 2:4, cols], v3(ekk[:, 0:256], 2), ALU.mult, [self.r_qkg, rekk], [rkpp])
        if getattr(self, "stop", 99) < 3.3:
            return
        bank, rb = self.nb()
        bv = self.bankb(bank)
        for p in range(2):
            self.tr(bv[:, p * 128:(p + 1) * 128], kpp[:, p * 128:(p + 1) * 128], [rkpp], [rb])
        kt, rkt = self.t1k()
        self.cp("dve", kt[:, 0:256], bv[:, 0:256], [rb], [rkt])
        if getattr(self, "stop", 99) < 3.4:
            return
        scs = [self.nb(), self.nb()]
        for h in range(4):
            p, hh = h // 2, h % 2
            hb = hh * 64
            self.mm(scs[hh][0][:, p * 128:(p + 1) * 128], kp[hb:hb + 64, p * 128:(p + 1) * 128],
                    qp[hb:hb + 64, p * 128:(p + 1) * 128], True, True, [rkp, rqp], [scs[hh][1]])
        am, ram = self.t1k()
        amv = am[:, :].rearrange("p (a b c) -> p a b c", a=2, b=2)
        for hh in range(2):
            self.tt(amv[:, :, hh, :], v3(scs[hh][0][:, 0:256], 2), self.triB[:, :].unsqueeze(1).to_broadcast([128, 2, 128]),
                    ALU.mult, [scs[hh][1], self.r_const], [ram])
        if getattr(self, "stop", 99) < 3.5:
            return
        ob, rob = self.nb()
        for h in range(4):
            p, hb = h // 2, (h % 2) * 64
            self.mm(ob[:, h * 128:(h + 1) * 128], am[:, h * 128:(h + 1) * 128], self.vc[:, t, h * 128:(h + 1) * 128],
                    True, tt_ == 0, [ram, self.r_vc[t]], [rob])
            if tt_ > 0:
                self.mm(ob[:, h * 128:(h + 1) * 128], qp[hb:hb + 64, p * 128:(p + 1) * 128],
                        self.Sb[hb:hb + 64, p * 128:(p + 1) * 128], False, True, [rqp, self.r_Sb], [rob])
        if getattr(self, "stop", 99) < 3.6:
            return
        ds, rds = self.nb()
        for p in range(2):
            self.mm(ds[:, p * 256:(p + 1) * 256], kt[:, p * 128:(p + 1) * 128], self.vc[:, t, p * 256:(p + 1) * 256],
                    True, True, [rkt, self.r_vc[t]], [rds])
        for p in range(2):
            for hh in range(2):
                rows = slice(hh * 64, hh * 64 + 64)
                dsv = ds[rows, p * 256 + hh * 128:p * 256 + hh * 128 + 128]
                sfv = self.Sf[rows, p * 128:(p + 1) * 128]
                if tt_ == 0:
                    self.cp("dve", sfv, dsv, [rds], [self.r_Sf])
                else:
                    self.stt(sfv, sfv, st[rows, 2 + p:3 + p], dsv, ALU.mult, ALU.add, [self.r_Sf, rst, rds], [self.r_Sf])
        self.cp("act", self.Sb[:, :], self.Sf[:, :], [self.r_Sf], [self.r_Sb])
        if getattr(self, "stop", 99) < 3.7:
            return
        of, rof = self.t2k()
        self.cp("act", of[:, :], ob[:, :], [rob], [rof])
        sq, rsq = self.t2k()
        self.tt(sq[:, :], of[:, :], of[:, :], ALU.mult, [rof], [rsq])
        st2, rst2 = self.small()
        self.red(st2[:, 0:4], v3(sq[:, :], 4), ALU.add, [rsq], [rst2])
        self.act(st2[:, 4:8], st2[:, 0:4], AF.Ln, [rst2, self.r_cst], [rst2], bias=self.cst[:, 1:2], scale=1.0 / 128)
        self.act(st2[:, 4:8], st2[:, 4:8], AF.Exp, [rst2], [rst2], scale=-0.5)
        self.tt(v3(of[:, :], 4), v3(of[:, :], 4), st2[:, 4:8].unsqueeze(2).to_broadcast([128, 4, 128]), ALU.mult,
                [rof, rst2], [rof])
        self.tt(v3(of[:, :], 4), v3(of[:, :], 4), self.rowbc[:, 2304:2432].unsqueeze(1).to_broadcast([128, 4, 128]),
                ALU.mult, [rof, rl], [rof])
        A, rA = self.t1k()
        self.tt(A[:, :], of[:, :], self.rq[:, t, :], ALU.mult, [rof, self.r_rq], [rA])
        bank, rb = self.nb()
        bv = self.bankb(bank)
        for c in range(4):
            self.tr(bv[:, c * 128:(c + 1) * 128], A[:, c * 128:(c + 1) * 128], [rA], [rb])
        self.cp("dve", self.big[:, 8:12, cols], v3(bv[:, 0:512], 4), [rb], self.r_big[8:12])

    def pool_tile(self, g, t):
        tt_ = g * 4 + t
        cols = slice(t * 128, (t + 1) * 128)
        bank, rb = self.nb()
        for gi in range(4):
            gs = slice(gi * 128, (gi + 1) * 128)
            if tt_ == 0:
                self.mm(bank[:, gs], self.u[:, 1 + t, gs], self.cpool[:, 8 + gi, :], True, True,
                        [self.r_u[1 + t], self.r_const], [rb])
            else:
                self.mm(bank[:, gs], self.u[:, 1 + t, gs], self.cpool[:, gi, :], True, False,
                        [self.r_u[1 + t], self.r_const], [rb])
                self.mm(bank[:, gs], self.u[:, t, gs], self.cpool[:, 4 + gi, :], False, True,
                        [self.r_u[t], self.r_const], [rb])
        pT, rpT = self.t1k()
        self.cp("act", pT[:, :], bank[:, :], [rb], [rpT])
        b2, rb2 = self.nb()
        for gi in range(4):
            gs = slice(gi * 128, (gi + 1) * 128)
            self.mm(b2[:, gs], self.wpool[:, gi, :], pT[:, gs], True, True, [self.r_layerb, rpT], [rb2])
        self.tt(self.big[:, 12:16, cols], v3(b2[:, :], 4), self.vecf[:, 16:20].unsqueeze(2).to_broadcast([128, 4, 128]),
                ALU.mult, [rb2, self.r_layer], self.r_big[12:16])

    def moba_group(self, g):
        qT = self.rq
        rq = self.r_rq
        self.memset("dve", self.selb[:, :, :], 0.0, [self.r_selb])
        for t in range(4):
            tt_ = g * 4 + t
            bt = tt_ // 2
            if bt < 4:
                continue
            cols = slice(t * 128, (t + 1) * 128)
            bank, rb = self.nb()
            for p in range(4):
                ps_ = bank[:, p * 32:(p + 1) * 32]
                self.mm(ps_, qT[:, p, cols], self.kmBD[:, p, :], True, True, [rq, self.r_kmBD], [rb])
            Ga, rGa = self.t2k()
            Gb, rGb = self.t2k()
            Gc, rGc = self.t2k()
            Gd, rGd = self.t2k()
            self.cp("dve", Ga[:, 0:128], bank[:, 0:128], [rb], [rGa])

            def V(x):
                return v3(x[:, 0:128], 8)[:, :, 0:bt]
            m, rm = self.small()

            def bc(col):
                return col.unsqueeze(2).to_broadcast([128, 8, bt])
            self.red(m[:, 0:8], V(Ga), ALU.max, [rGa], [rm])
            self.tt(V(Gb), V(Ga), bc(m[:, 0:8]), ALU.is_ge, [rGa, rm], [rGb])
            self.stt(V(Gc), V(Gb), NEGBIG, V(Ga), ALU.mult, ALU.add, [rGb, rGa], [rGc])
            m2, rm2 = self.small()
            self.red(m2[:, 0:8], V(Gc), ALU.max, [rGc], [rm2])
            self.tt(V(Gb), V(Gc), bc(m2[:, 0:8]), ALU.is_ge, [rGc, rm2], [rGb])
            self.stt(V(Gd), V(Gb), NEGBIG, V(Gc), ALU.mult, ALU.add, [rGb, rGc], [rGd])
            m3, rm3 = self.small()
            self.red(m3[:, 0:8], V(Gd), ALU.max, [rGd], [rm3])
            self.tt(V(Gb), V(Ga), bc(m3[:, 0:8]), ALU.is_lt, [rGa, rm3], [rGb])
            self.ts(v3(self.selb[:, t, :], 8)[:, :, 0:bt], V(Gb), NEGV, ALU.mult, [rGb], [self.r_selb])

        if self.dbg and self.cur_li == 0:
            self.S.dma("sp", self.dbg_selb[g], self.selb[:, :, :], reads=[self.r_selb], writes=[self.r_dbg])
        for h in range(8):
            p, hb = h // 2, (h % 2) * 64
            bank, rb = self.nb()
            bv = self.bankb(bank)
            for t in range(4):
                self.tr(bv[hb:hb + 16, t * 128:(t + 1) * 128], self.selb[:, t, h * 16:(h + 1) * 16], [self.r_selb], [rb])
            selT, rselT = self.selTb[h % 2], self.r_selTb[h % 2]
            self.cp("dve", selT[hb:hb + 16, :], bv[hb:hb + 16, 0:512], [rb], [rselT])
            if self.dbg and self.cur_li == 0 and g == 2 and h == 2:
                self.S.dma("sp", self.dbg_selT[:, :], selT[hb:hb + 16, :], reads=[rselT], writes=[self.r_dbg3])
            oi = 6 + (h % 2)
            ob, rob = self.bank[oi], self.r_bank[oi]
            self.memset("dve", ob[:, 0:260], 0.0, [rob])
            for i in range(4 * g + 4):
                n = i // 2
                js = [j for j in range(4) if 4 * g + j >= i and 4 * g + j - i <= WIN[h]]
                if not js:
                    continue
                own = [j for j in js if (4 * g + j) // 2 == n]
                past = [j for j in js if (4 * g + j) // 2 > n]
                sb_, rsb = self.nb()
                kap = self.kTm[hb:hb + 64, p, i * 128:(i + 1) * 128]
                rk = self.r_kTm[i // 4]
                if past:
                    pc = slice(past[0] * 128, (past[-1] + 1) * 128)
                    self.mm(sb_[:, pc], self.e16[hb:hb + 16, n, :], selT[hb:hb + 16, pc], True, False,
                            [self.r_const, rselT], [rsb])
                    self.mm(sb_[:, pc], kap, qT[hb:hb + 64, p, pc], False, True, [rk, rq], [rsb])
                if own:
                    jj = list(own)
                    if 4 * g + jj[0] == i:
                        dc = slice(jj[0] * 128, (jj[0] + 1) * 128)
                        self.mm(sb_[:, dc], kap, qT[hb:hb + 64, p, dc], True, False, [rk, rq], [rsb])
                        self.mm(sb_[:, dc], self.identB[:, :], self.negtri[:, :], False, True, [self.r_const], [rsb])
                        jj = jj[1:]
                    if jj:
                        oc = slice(jj[0] * 128, (jj[-1] + 1) * 128)
                        self.mm(sb_[:, oc], kap, qT[hb:hb + 64, p, oc], True, True, [rk, rq], [rsb])
                PT, rPT = self.t1k()
                if h < 2:
                    for j in js:
                        d = 4 * g + j + 1 - i
                        jc = slice(j * 128, (j + 1) * 128)
                        self.act(PT[:, jc], sb_[:, jc], AF.Exp, [rsb, self.r_constf], [rPT],
                                 bias=self.alibi[:, h * 33 + d:h * 33 + d + 1], scale=0.125)
                else:
                    d = 4 * g + 4 - i
                    ac = slice(js[0] * 128, (js[-1] + 1) * 128)
                    self.act(PT[:, ac], sb_[:, ac], AF.Exp, [rsb, self.r_constf], [rPT],
                             bias=self.alibi[:, h * 33 + d:h * 33 + d + 1], scale=0.125)
                if self.dbg and self.cur_li == 0 and g == 2 and h == 2 and i == 7:
                    self.S.dma("sp", self.dbg_PT[:, :], PT[:, :], reads=[rPT], writes=[self.r_dbg4])
                for j in js:
                    jt = 4 * g + j
                    first_i = max(0, jt - WIN[h])
                    self.mm(ob[:, j * 65:(j + 1) * 65], PT[:, j * 128:(j + 1) * 128], self.Vaug[:, i, h, :],
                            False, False, [rPT, self.r_V[i]], [rob], skip=True)
            rinv, rri = self.small()
            obv = v3(ob[:, 0:260], 4)
            self.S.op("dve", lambda e, o=rinv[:, 0:4], i=obv[:, :, 64]: e.reciprocal(out=o, in_=i), [rob], [rri])
            self.tt(self.vc[:, :, h * 64:(h + 1) * 64], obv[:, :, 0:64], rinv[:, 0:4].unsqueeze(2).to_broadcast([128, 4, 64]),
                    ALU.mult, [rob, rri], self.r_vc)
        if self.dbg and self.cur_li == 0:
            self.S.dma("sp", self.dbg_C[g], self.vc[:, :, :], reads=self.r_vc, writes=[self.r_dbg2])
        for j in range(4):
            bank, rb = self.nb()
            bv = self.bankb(bank)
            for c in range(4):
                self.tr(bv[:, c * 128:(c + 1) * 128], self.vc[:, j, c * 128:(c + 1) * 128], [self.r_vc[j]], [rb])
            self.cp("dve", self.big[:, 16:20, j * 128:(j + 1) * 128], v3(bv[:, 0:512], 4), [rb], self.r_big[16:20])

    def mix_out(self, li, g):
        for c in range(8):
            wb, rw = self.load_block(li, "M%d" % c)
            Wg = wb[:, 0:3072].rearrange("p (k i c) -> p k i c", k=8, i=3)
            Wb = wb[:, 3072:4608].rearrange("p (i k c) -> p i k c", i=3, k=4)
            acc, racc = self.t2k()
            for i in range(3):
                bg, rbg = self.nb()
                for kc in range(8):
                    self.mm(bg[:, :], Wg[:, kc, i, :], self.hT[:, kc, :], kc == 0, kc == 7, [rw, self.r_hT], [rbg])
                sg, rsg = self.t2k()
                self.act(sg[:, :], bg[:, :], AF.Sigmoid, [rbg], [rsg])
                by, rby = self.nb()
                for kc in range(4):
                    ch = 8 + 4 * i + kc
                    self.mm(by[:, :], Wb[:, i, kc, :], self.big[:, ch, :], kc == 0, kc == 3, [rw, self.r_big[ch]], [rby])
                if i == 0:
                    self.tt(acc[:, :], by[:, :], sg[:, :], ALU.mult, [rby, rsg], [racc])
                else:
                    self.tt(sg[:, :], by[:, :], sg[:, :], ALU.mult, [rby, rsg], [rsg])
                    if i == 1:
                        self.tt(acc[:, :], acc[:, :], sg[:, :], ALU.add, [racc, rsg], [racc])
                    else:
                        self.tt(self.big[:, c, :], acc[:, :], sg[:, :], ALU.add, [racc, rsg], [self.r_big[c]])
            self.done_block()
        wo = [self.load_block(li, "O%d" % hf) for hf in range(2)]
        mo = self.qkg[:, 0:2, :].rearrange("p a b -> p (a b)")
        rmo = self.r_qkg
        for t in range(4):
            for hf in range(2):
                W = v3(wo[hf][0][:, 0:4096], 8)
                bank, rb = self.nb()
                for kc in range(8):
                    self.mm(bank[:, :], self.big[:, kc, t * 128:(t + 1) * 128], W[:, kc, :], kc == 0, kc == 7,
                            [wo[hf][1], self.r_big[kc]], [rb])
                self.cp("act", mo[:, hf * 512:(hf + 1) * 512], bank[:, :], [rb], [rmo])
            self.post_norm_residual(mo, rmo, self.rowbc[:, 0:1024], self.xg[:, t, :], self.r_xg[t])
        self.done_block()
        self.done_block()

    def ffn(self, li, g):
        self.norm_T(8)
        self.rot_set = [0, 1, 2, 3]
        for j in range(11):
            wb, rw = self.load_block(li, "GU%d" % j)
            W = wb[:, 0:4096].rearrange("p (k g c) -> p k g c", k=8, g=2)
            for m in range(2):
                bg, rbg = self.nb()
                for kc in range(8):
                    self.mm(bg[:, :], W[:, kc, 0, m * 128:(m + 1) * 128], self.hT[:, kc, :], kc == 0, kc == 7,
                            [rw, self.r_hT], [rbg])
                bu, rbu = self.nb()
                for kc in range(8):
                    self.mm(bu[:, :], W[:, kc, 1, m * 128:(m + 1) * 128], self.hT[:, kc, :], kc == 0, kc == 7,
                            [rw, self.r_hT], [rbu])
                sg, rsg = self.t2k()
                self.act(sg[:, :], bg[:, :], AF.Silu, [rbg], [rsg])
                ch = 2 * j + m
                self.tt(self.big[:, ch, :], bu[:, :], sg[:, :], ALU.mult, [rbu, rsg], [self.r_big[ch]])
            self.done_block()
        mo = self.qkg[:, 0:2, :].rearrange("p a b -> p (a b)")
        rmo = self.r_qkg
        for ps in range(2):
            for j in range(11):
                wb, rw = self.load_block(li, "DN%d" % j)
                Wd = v3(wb[:, 0:2048], 2)
                for m in range(2):
                    ch = 2 * j + m
                    for tl in range(2):
                        t = 2 * ps + tl
                        for hf in range(2):
                            bi = 4 + tl * 2 + hf
                            self.mm(self.bank[bi][:, :], self.big[:, ch, t * 128:(t + 1) * 128],
                                    Wd[:, m, hf * 512:(hf + 1) * 512], ch == 0, ch == 21, [self.r_big[ch], rw],
                                    [self.r_bank[bi]])
                self.done_block()
            for tl in range(2):
                t = 2 * ps + tl
                for hf in range(2):
                    bi = 4 + tl * 2 + hf
                    self.cp("act", mo[:, hf * 512:(hf + 1) * 512], self.bank[bi][:, :], [self.r_bank[bi]], [rmo])
                self.post_norm_residual(mo, rmo, self.rowbc[:, 1024:2048], self.xg[:, t, :], self.r_xg[t])
        self.rot_set = [0, 1, 2, 3, 4, 5]


def make_consts():
    s = np.arange(128)[:, None]
    t = np.arange(128)[None, :]
    c = {}
    c["c_ident"] = np.eye(128, dtype=np.float32)
    c["c_tri"] = (s <= t).astype(np.float32)
    c["c_negtri"] = np.where(s <= t, 0.0, NEGV).astype(np.float32)
    pm = np.zeros((12, 128, 128), np.float32)
    for gi, w in enumerate((2, 4, 8, 16)):
        dlt = t - s
        pm[gi] = np.where((dlt >= 0) & (dlt < w), 1.0 / w, 0.0) - (s == t)
        dprev = t + 128 - s
        pm[4 + gi] = np.where(dprev < w, 1.0 / w, 0.0)
        cnt = np.minimum(t + 1, w).astype(np.float32)
        pm[8 + gi] = np.where((dlt >= 0) & (dlt < w), 1.0 / cnt, 0.0) - (s == t)
    c["c_pool"] = pm
    e = np.zeros((128, 16, 128), np.float32)
    for n in range(16):
        e[n, n, :] = 1.0
        e[64 + n, n, :] = 1.0
    c["c_e16"] = e.reshape(128, 16 * 128)
    al = np.zeros((128, 8, 33), np.float32)
    pp = np.arange(128, dtype=np.float64)[:, None]
    dd = np.arange(33, dtype=np.float64)[None, :]
    for h in range(8):
        al[:, h, :] = SLOPES[h] * (pp + 1.0 - 128.0 * dd)
    c["c_alibi"] = al.reshape(128, 8 * 33)
    return c


def layer_inputs(inp, ls):
    ls = list(ls)
    d = {}
    for k in ("w_in", "gla_w_g2", "pool_w", "w_branch_a", "w_branch_b", "w_branch_c", "w_out",
              "ffn_w_gate", "ffn_w_up", "ffn_w_down"):
        d[k] = np.ascontiguousarray(np.asarray(inp[k], dtype=np.float32)[ls])
    vecf = np.zeros((len(ls), 128, 20), np.float32)
    rowbc = np.zeros((len(ls), 128, 2432), np.float32)
    for i, l in enumerate(ls):
        vecf[i, :, 0:8] = np.asarray(inp["norm_mix_pre"][l]).reshape(8, 128).T
        vecf[i, :, 8:16] = np.asarray(inp["norm_ffn_pre"][l]).reshape(8, 128).T
        vecf[i, :, 16:20] = np.asarray(inp["pool_scale"][l]).reshape(4, 128).T
        rowbc[i, :, 0:1024] = np.asarray(inp["norm_mix_post"][l])[None, :]
        rowbc[i, :, 1024:2048] = np.asarray(inp["norm_ffn_post"][l])[None, :]
        rowbc[i, :, 2048:2304] = np.asarray(inp["gla_b_g"][l])[None, :]
        rowbc[i, :, 2304:2432] = np.asarray(inp["gla_norm"][l])[None, :]
    d["vecf"] = vecf
    d["rowbc"] = rowbc
    d.update(make_consts())
    return d


_PROG_CACHE = {}


STOP_STAGE = 99


def get_prog(T, nl):
    key = (T, nl)
    if key not in _PROG_CACHE:
        p = Prog(T, nl)
        p.stop = STOP_STAGE
        _PROG_CACHE[key] = p.build()
    return _PROG_CACHE[key]


def run_layers(xs, inp, ls):
    T = xs[0].shape[0]
    nc = get_prog(T, len(ls))
    shared = layer_inputs(inp, ls)
    in_maps = []
    for x in xs:
        m = dict(shared)
        m["x"] = np.ascontiguousarray(x, dtype=np.float32)
        in_maps.append(m)
    res = run_bass_kernel_spmd(nc, in_maps, core_ids=list(range(len(xs))))
    return [np.asarray(r["y"]) for r in res.results]


FUSED = True


def kernel(**inputs):
    x = np.asarray(inputs["x"], dtype=np.float32)
    B = x.shape[0]
    xs = [x[b] for b in range(B)]
    if FUSED:
        ys = run_layers(xs, inputs, [0, 1])
    else:
        ys = run_layers(xs, inputs, [0])
        ys = run_layers(ys, inputs, [1])
    return np.stack(ys, axis=0).astype(np.float32)
```

```python
import contextlib
import numpy as np
import concourse.bass as bass
import concourse.mybir as mybir
from concourse.bass_utils import run_bass_kernel_spmd

F32 = mybir.dt.float32
BF16 = mybir.dt.bfloat16
AF = mybir.ActivationFunctionType
ALU = mybir.AluOpType
AX = mybir.AxisListType

D = 1024
DIN = 6672
DFF = 2816
EPS = 1e-6
NEGV = -30000.0
NEGBIG = -1.0e30
WIN = [2, 4, 8, 16, 999, 999, 999, 999]
SLOPES = [2.0 ** (-(h + 1)) for h in range(8)]
WBUF_EL = 4608
N_WBUF = 3
LOADQ = "act"
SAME_ENG_SYNC = True


class DSem:
    __slots__ = ("key", "cnt")

    def __init__(self, key):
        self.key = key
        self.cnt = 0


class Res:
    __slots__ = ("name", "w", "r", "dsem", "excl")

    def __init__(self, name, excl=False):
        self.name = name
        self.w = None
        self.r = {}
        self.dsem = None
        self.excl = excl


class Sched:
    ENGS = ("pe", "act", "dve", "pool", "sp")

    def __init__(self, nc, es):
        self.nc = nc
        self.es = es
        self.q = {e: [] for e in self.ENGS}
        self.sems = {}
        for e in self.ENGS:
            self.sems[e] = es.enter_context(nc.semaphore("sem_" + e))
        self.cnt = {e: 0 for e in self.ENGS}
        self.seen = {e: {} for e in self.ENGS}
        self.ndsem = 0

    def new_dsem(self):
        k = "dma%d" % self.ndsem
        self.ndsem += 1
        self.sems[k] = self.es.enter_context(self.nc.semaphore("sem_" + k))
        return DSem(k)

    def _waits(self, eng, reads, writes):
        seen = self.seen[eng]
        best = {}

        def add(k, v):
            if k == eng and (eng == "pe" or not SAME_ENG_SYNC):
                return
            if seen.get(k, 0) >= v:
                return
            if best.get(k, 0) < v:
                best[k] = v
        for r in reads:
            if r.w is not None:
                add(*r.w)
            if r.excl:
                for k, v in r.r.items():
                    if k != eng:
                        add(k, v)
        for w in writes:
            if w.w is not None:
                add(*w.w)
            for k, v in w.r.items():
                add(k, v)
        out = []
        for k, v in best.items():
            seen[k] = v
            out.append((k, v))
        return out

    def op(self, eng, fn, reads=(), writes=()):
        waits = self._waits(eng, reads, writes)
        self.cnt[eng] += 1
        tokv = self.cnt[eng]
        for r in reads:
            if r.r.get(eng, 0) < tokv:
                r.r[eng] = tokv
        for w in writes:
            w.w = (eng, tokv)
            w.r = {}
        self.q[eng].append((waits, fn, eng, 1))

    def dma(self, qeng, out, in_, reads=(), writes=(), nowait=False):
        waits = [] if nowait else self._waits(qeng, reads, writes)
        tgt = writes[0]
        if tgt.dsem is None:
            tgt.dsem = self.new_dsem()
        ds = tgt.dsem
        ds.cnt += 16
        tok = (ds.key, ds.cnt)
        for r in reads:
            if r.r.get(tok[0], 0) < tok[1]:
                r.r[tok[0]] = tok[1]
        for w in writes:
            w.w = tok
            w.r = {}

        def fn(e, out=out, in_=in_):
            return e.dma_start(out=out, in_=in_)
        self.q[qeng].append((waits, fn, ds.key, 16))

    def final_wait(self, eng, ress):
        waits = self._waits(eng, ress, ())
        self.q[eng].append((waits, None, None, 0))

    def replay(self):
        nc = self.nc
        sems = self.sems
        q = self.q
        with nc.Block() as block:
            def run(e, name):
                for waits, fn, isem, iv in q[name]:
                    for k, v in waits:
                        e.wait_ge(sems[k], v)
                    if fn is None:
                        continue
                    ins = fn(e)
                    if isem is not None:
                        ins.then_inc(sems[isem], iv)

            @block.tensor
            def _(e):
                run(e, "pe")

            @block.scalar
            def _(e):
                run(e, "act")

            @block.vector
            def _(e):
                run(e, "dve")

            @block.gpsimd
            def _(e):
                run(e, "pool")

            @block.sync
            def _(e):
                run(e, "sp")


def v3(ap, a):
    return ap.rearrange("p (a b) -> p a b", a=a)


class Prog:
    def __init__(self, T, nlayers, dbg=False):
        self.T = T
        self.NL = nlayers
        self.NT = T // 128
        self.NG = T // 512
        self.dbg = dbg
        self.nc = bass.Bass("TRN2", target_bir_lowering=False)

    def sb(self, name, shape, dt):
        return self.es.enter_context(self.nc.sbuf_tensor(name, shape, dt))

    def mm(self, out, lhsT, rhs, start, stop, reads, writes, skip=False):
        if skip:
            self.S.op("pe", lambda e, o=out, l=lhsT, r=rhs, s=start, p=stop: e.matmul(o, lhsT=l, rhs=r, start=s, stop=p,
                                                                                     skip_group_check=True), reads, writes)
        else:
            self.S.op("pe", lambda e, o=out, l=lhsT, r=rhs, s=start, p=stop: e.matmul(o, lhsT=l, rhs=r, start=s, stop=p),
                      reads, writes)

    def tr(self, out, in_, reads, writes):
        idn = self.identB[:, :]
        self.S.op("pe", lambda e, o=out, i=in_, d=idn: e.transpose(o, i, d), list(reads) + [self.r_const], writes)

    def act(self, out, in_, func, reads, writes, bias=None, scale=None, accum_out=None):
        kw = {}
        if bias is not None:
            kw["bias"] = bias
        if scale is not None:
            kw["scale"] = scale
        if accum_out is not None:
            kw["accum_out"] = accum_out
        self.S.op("act", lambda e, o=out, i=in_, f=func, kw=kw: e.activation(out=o, in_=i, func=f, **kw), reads, writes)

    def tt(self, out, in0, in1, op, reads, writes, eng="dve"):
        self.S.op(eng, lambda e, o=out, a=in0, b=in1, op=op: e.tensor_tensor(out=o, in0=a, in1=b, op=op), reads, writes)

    def ts(self, out, in0, s1, op0, reads, writes, s2=None, op1=None, eng="dve"):
        if op1 is None:
            self.S.op(eng, lambda e, o=out, a=in0, s1=s1, op0=op0: e.tensor_scalar(out=o, in0=a, scalar1=s1, scalar2=0.0, op0=op0,
                                                                                  op1=ALU.add), reads, writes)
        else:
            self.S.op(eng, lambda e, o=out, a=in0, s1=s1, s2=s2, op0=op0, op1=op1:
                      e.tensor_scalar(out=o, in0=a, scalar1=s1, scalar2=s2, op0=op0, op1=op1), reads, writes)

    def stt(self, out, in0, scalar, in1, op0, op1, reads, writes, eng="dve"):
        self.S.op(eng, lambda e, o=out, a=in0, s=scalar, b=in1, op0=op0, op1=op1:
                  e.scalar_tensor_tensor(out=o, in0=a, scalar=s, in1=b, op0=op0, op1=op1), reads, writes)

    def red(self, out, in_, op, reads, writes):
        self.S.op("dve", lambda e, o=out, i=in_, op=op: e.tensor_reduce(out=o, in_=i, axis=AX.X, op=op), reads, writes)

    def cp(self, eng, out, in_, reads, writes):
        if eng == "act":
            self.S.op("act", lambda e, o=out, i=in_: e.copy(out=o, in_=i), reads, writes)
        else:
            self.S.op(eng, lambda e, o=out, i=in_: e.tensor_copy(out=o, in_=i), reads, writes)

    def memset(self, eng, ap, val, writes):
        self.S.op(eng, lambda e, a=ap, v=val: e.memset(a, v), (), writes)

    def nb(self):
        i = self.rot_set[self.rot_i % len(self.rot_set)]
        self.rot_i += 1
        return self.bank[i], self.r_bank[i]

    def bankb(self, bank):
        return bank[:, :].bitcast(BF16)

    def t2k(self):
        i = self.t2k_i % len(self.T2K)
        self.t2k_i += 1
        return self.T2K[i], self.r_T2K[i]

    def t1k(self):
        i = self.t1k_i % len(self.T1K)
        self.t1k_i += 1
        return self.T1K[i], self.r_T1K[i]

    def small(self):
        i = self.sm_i % len(self.SM)
        self.sm_i += 1
        return self.SM[i], self.r_SM[i]

    def block_seq(self):
        seq = []
        for li in range(self.NL):
            for g in range(self.NG):
                seq += [(li, "B%d" % b) for b in range(7)]
                seq += [(li, "M%d" % c) for c in range(8)]
                seq += [(li, "O0"), (li, "O1")]
                seq += [(li, "GU%d" % j) for j in range(11)]
                seq += [(li, "DN%d" % j) for j in range(11)] * 2
        return seq

    def prefetch(self):
        if self.wpos_load >= len(self.wseq):
            return
        i = self.wpos_load % N_WBUF
        ap, res, nel = self.blocks[self.wseq[self.wpos_load]]
        self.wpos_load += 1
        self.S.dma(LOADQ, self.wbuf[i][:, 0:nel], ap[:, 0:nel], reads=[res], writes=[self.r_wbuf[i]])

    def load_block(self, li, name):
        assert self.wseq[self.wpos_use] == (li, name), (self.wseq[self.wpos_use], li, name)
        while self.wpos_load <= self.wpos_use:
            self.prefetch()
        i = self.wpos_use % N_WBUF
        self.wpos_use += 1
        return self.wbuf[i], self.r_wbuf[i]

    def done_block(self):
        self.prefetch()

    def build(self):
        nc = self.nc
        T, NL, NT, NG = self.T, self.NL, self.NT, self.NG
        with contextlib.ExitStack() as es:
            self.es = es
            self.S = Sched(nc, es)
            S = self.S

            def din(name, shape):
                return nc.dram_tensor(name, shape, F32, kind="ExternalInput").ap()
            self.x_in = din("x", [T, D])
            self.w_in = din("w_in", [NL, D, DIN])
            self.w_g2 = din("gla_w_g2", [NL, 16, 256])
            self.pool_w = din("pool_w", [NL, 4, 128, 128])
            self.w_br = [din("w_branch_" + c, [NL, 512, D]) for c in "abc"]
            self.w_out = din("w_out", [NL, D, D])
            self.w_gate = din("ffn_w_gate", [NL, D, DFF])
            self.w_up = din("ffn_w_up", [NL, D, DFF])
            self.w_down = din("ffn_w_down", [NL, DFF, D])
            self.vecf_d = din("vecf", [NL, 128, 20])
            self.rowbc_d = din("rowbc", [NL, 128, 2432])
            c_ident = din("c_ident", [128, 128])
            c_tri = din("c_tri", [128, 128])
            c_negtri = din("c_negtri", [128, 128])
            c_pool = din("c_pool", [12, 128, 128])
            c_e16 = din("c_e16", [128, 16 * 128])
            c_alibi = din("c_alibi", [128, 8 * 33])
            self.y = nc.dram_tensor("y", [T, D], F32, kind="ExternalOutput").ap()
            self.xres = nc.dram_tensor("xres", [T, D], F32).ap() if NL > 1 else None
            if self.dbg:
                self.dbg_selb = nc.dram_tensor("dbg_selb", [NG, 128, 4, 128], BF16, kind="ExternalOutput").ap()
                self.r_dbg = Res("dbg")
                self.dbg_C = nc.dram_tensor("dbg_C", [NG, 128, 4, 512], BF16, kind="ExternalOutput").ap()
                self.r_dbg2 = Res("dbg2")
                self.dbg_selT = nc.dram_tensor("dbg_selT", [16, 512], BF16, kind="ExternalOutput").ap()
                self.r_dbg3 = Res("dbg3")
                self.dbg_PT = nc.dram_tensor("dbg_PT", [128, 512], BF16, kind="ExternalOutput").ap()
                self.r_dbg4 = Res("dbg4")
            self.r_xrow = [[Res("xrow%d_%d" % (l, t)) for t in range(NT)] for l in range(NL + 1)]
            st_sems = [S.new_dsem() for _ in range(4)]
            for l in range(1, NL + 1):
                for t in range(NT):
                    self.r_xrow[l][t].dsem = st_sems[t % 4]

            sb = self.sb
            self.identB = sb("identB", [128, 128], BF16)
            self.triF = sb("triF", [128, 128], F32)
            self.triB = sb("triB", [128, 128], BF16)
            self.negtri = sb("negtri", [128, 128], BF16)
            self.cpool = sb("cpool", [128, 12, 128], BF16)
            self.e16 = sb("e16", [128, 16, 128], BF16)
            self.alibi = sb("alibi", [128, 8 * 33], F32)
            self.cst = sb("cst", [128, 4], F32)
            self.r_const = Res("const")
            self.r_constf = Res("constf")
            self.vecf = sb("vecf_s", [128, 20], F32)
            self.rowbc = sb("rowbc_s", [128, 2432], F32)
            self.wg2 = sb("wg2", [16, 256], BF16)
            self.wpool = sb("wpool", [128, 4, 128], BF16)
            self.r_layer = Res("layerconst")
            self.r_layerb = Res("layerconstb")
            self.kTm = sb("kTm", [128, 4, T], BF16)
            self.r_kTm = [Res("kTm%d" % g) for g in range(NG)]
            self.Vaug = sb("Vaug", [128, NT, 8, 65], BF16)
            self.r_V = [Res("V%d" % t) for t in range(NT)]
            self.kmBD = sb("kmBD", [128, 4, 32], BF16)
            self.kmF = sb("kmF", [128, 4, 32], F32)
            self.r_kmBD = Res("kmBD")
            self.ksum = sb("ksum", [128, 8], F32)
            self.r_ksum = Res("ksum")
            self.Sf = sb("Sf", [128, 256], F32)
            self.Sb = sb("Sb", [128, 256], BF16)
            self.r_Sf = Res("Sf")
            self.r_Sb = Res("Sb")
            self.xg = sb("xg", [128, 4, D], F32)
            self.r_xg = [Res("xg%d" % t) for t in range(4)]
            self.hT = sb("hT", [128, 8, 512], BF16)
            self.r_hT = Res("hT")
            self.xs = sb("xs", [128, D], BF16)
            self.r_xs = Res("xs")
            self.wbuf = [sb("wbuf%d" % i, [128, WBUF_EL], BF16) for i in range(N_WBUF)]
            self.r_wbuf = [Res("wbuf%d" % i) for i in range(N_WBUF)]
            self.qkg = sb("qkg", [128, 4, 512], F32)
            self.r_qkg = Res("qkg")
            self.g1T = sb("g1T", [16, 512], BF16)
            self.r_g1T = Res("g1T")
            self.vc = sb("vc", [128, 4, 512], BF16)
            self.r_vc = [Res("vc%d" % t) for t in range(4)]
            self.rq = sb("rq", [128, 4, 512], BF16)
            self.r_rq = Res("rq")
            self.u = sb("u", [128, 5, 512], BF16)
            self.r_u = [Res("u%d" % t) for t in range(5)]
            self.selb = sb("selb", [128, 4, 128], BF16)
            self.selTb = [sb("selT%d" % i, [128, 512], BF16) for i in range(2)]
            self.r_selTb = [Res("selT%d" % i) for i in range(2)]
            self.r_selb = Res("selb")
            self.big = sb("big", [128, 22, 512], BF16)
            self.r_big = [Res("big%d" % c) for c in range(22)]
            self.T2K = [sb("t2k%d" % i, [128, 512], F32) for i in range(5)]
            self.r_T2K = [Res("t2k%d" % i) for i in range(5)]
            self.T1K = [sb("t1k%d" % i, [128, 512], BF16) for i in range(6)]
            self.r_T1K = [Res("t1k%d" % i) for i in range(6)]
            self.SM = [sb("sm%d" % i, [128, 8], F32) for i in range(8)]
            self.r_SM = [Res("sm%d" % i) for i in range(8)]
            self.bank = [es.enter_context(nc.psum_tensor("bank%d" % i, [128, 512], F32)) for i in range(8)]
            self.r_bank = [Res("bank%d" % i, excl=True) for i in range(8)]
            self.rot_set = [0, 1, 2, 3, 4, 5]
            self.rot_i = self.t2k_i = self.t1k_i = self.sm_i = self.wb_i = 0
            print("sbuf bytes remaining after alloc", nc.sbuf_bytes_remaining)

            rc = self.r_const
            S.dma("pool", self.identB[:, :], c_ident[:, :], writes=[rc])
            S.dma("sp", self.triF[:, :], c_tri[:, :], writes=[self.r_constf])
            S.dma("pool", self.triB[:, :], c_tri[:, :], writes=[rc])
            S.dma("pool", self.negtri[:, :], c_negtri[:, :], writes=[rc])
            S.dma("pool", self.cpool[:, :, :], c_pool.rearrange("m s t -> s m t"), writes=[rc])
            S.dma("pool", self.e16[:, :, :], c_e16.rearrange("p (n s) -> p n s", n=16), writes=[rc])
            S.dma("sp", self.alibi[:, :], c_alibi[:, :], writes=[self.r_constf])
            r_cst = Res("cst")
            self.r_cst = r_cst
            self.memset("dve", self.cst[:, 0:1], 1.0, [r_cst])
            self.memset("dve", self.cst[:, 1:2], EPS, [r_cst])
            self.memset("dve", self.cst[:, 2:3], 0.0, [r_cst])
            self.memset("dve", self.Vaug[:, :, :, 64:65], 1.0, self.r_V)

            self.blocks = {}
            self.stage_res = {}
            for li in range(NL):
                self.cast_layer(li)

            self.wseq = self.block_seq()
            self.wpos_load = self.wpos_use = 0
            for _ in range(N_WBUF):
                self.prefetch()
            for li in range(NL):
                self.layer(li)

            outs = list(self.r_xrow[NL])
            if self.dbg:
                outs.append(self.r_dbg)
                outs.append(self.r_dbg2)
                outs.append(self.r_dbg3)
                outs.append(self.r_dbg4)
            S.final_wait("sp", outs)
            S.replay()
        return nc

    def new_block(self, li, name, nel):
        ap = self.nc.dram_tensor("ws_%d_%s" % (li, name), [128, nel], BF16).ap()
        stage = 0 if name[0] == "B" else (1 if name[0] in "MO" else 2)
        key = (li, stage)
        if key not in self.stage_res:
            self.stage_res[key] = Res("ws_%d_s%d" % key)
        res = self.stage_res[key]
        self.blocks[(li, name)] = (ap, res, nel)
        return ap, res

    def cast_layer(self, li):
        S = self.S
        wv = self.w_in[li].rearrange("(k p) n -> p k n", p=128)
        ap, res = self.new_block(li, "B0", 4224)
        d = v3(ap[:, 0:4224], 8)
        S.dma("pool", d[:, :, 0:512], wv[:, :, 0:512], writes=[res], nowait=True)
        S.dma("pool", d[:, :, 512:528], wv[:, :, 1024:1040], writes=[res], nowait=True)
        for bi, c0 in ((1, 512), (2, 1040), (3, 1552), (4, 2064), (5, 2576), (6, 3088)):
            ap, res = self.new_block(li, "B%d" % bi, 4096)
            S.dma("pool", v3(ap[:, 0:4096], 8), wv[:, :, c0:c0 + 512], writes=[res], nowait=True)
        for c in range(8):
            ap, res = self.new_block(li, "M%d" % c, 4608)
            dg = ap[:, 0:3072].rearrange("p (k i c) -> p k i c", k=8, i=3)
            db = ap[:, 3072:4608].rearrange("p (i k c) -> p i k c", i=3, k=4)
            for i in range(3):
                c0 = 3600 + i * 1024 + c * 128
                S.dma("pool", dg[:, :, i, :], wv[:, :, c0:c0 + 128], writes=[res], nowait=True)
                bv = self.w_br[i][li].rearrange("(k p) n -> p k n", p=128)
                S.dma("pool", db[:, i, :, :], bv[:, :, c * 128:(c + 1) * 128], writes=[res], nowait=True)
        ov = self.w_out[li].rearrange("(k p) n -> p k n", p=128)
        for hf in range(2):
            ap, res = self.new_block(li, "O%d" % hf, 4096)
            S.dma("pool", v3(ap[:, 0:4096], 8), ov[:, :, hf * 512:(hf + 1) * 512], writes=[res], nowait=True)
        gv = self.w_gate[li].rearrange("(k p) n -> p k n", p=128)
        uv = self.w_up[li].rearrange("(k p) n -> p k n", p=128)
        for j in range(11):
            ap, res = self.new_block(li, "GU%d" % j, 4096)
            dd = ap[:, 0:4096].rearrange("p (k g c) -> p k g c", k=8, g=2)
            S.dma("pool", dd[:, :, 0, :], gv[:, :, j * 256:(j + 1) * 256], writes=[res], nowait=True)
            S.dma("pool", dd[:, :, 1, :], uv[:, :, j * 256:(j + 1) * 256], writes=[res], nowait=True)
        for j in range(11):
            ap, res = self.new_block(li, "DN%d" % j, 2048)
            src = self.w_down[li][j * 256:(j + 1) * 256, :].rearrange("(m p) n -> p m n", p=128)
            S.dma("pool", v3(ap[:, 0:2048], 2), src, writes=[res], nowait=True)

    def layer(self, li):
        S = self.S
        rl = self.r_layer
        S.dma("sp", self.vecf[:, :], self.vecf_d[li], writes=[rl])
        S.dma("sp", self.rowbc[:, :], self.rowbc_d[li], writes=[rl])
        S.dma("pool", self.wg2[:, :], self.w_g2[li], writes=[self.r_layerb])
        S.dma("pool", self.wpool[:, :, :], self.pool_w[li].rearrange("g c d -> c g d"), writes=[self.r_layerb])
        self.memset("dve", self.kmBD[:, :, :], 0.0, [self.r_kmBD])
        self.memset("dve", self.kmF[:, :, :], 0.0, [self.r_kmBD])
        self.cur_li = li
        for g in range(self.NG):
            self.group(li, g)

    def xsrc(self, li):
        return self.x_in if li == 0 else self.xres

    def xdst(self, li):
        return self.y if li == self.NL - 1 else self.xres

    def norm_T(self, nwc):
        for t in range(4):
            xt = self.xg[:, t, :]
            rx = self.r_xg[t]
            st, rst = self.small()
            self.act(self.xs[:, :], xt, AF.Square, [rx], [self.r_xs, rst], accum_out=st[:, 0:1])
            self.act(st[:, 1:2], st[:, 0:1], AF.Ln, [rst, self.r_cst], [rst], bias=self.cst[:, 1:2], scale=1.0 / D)
            self.act(st[:, 1:2], st[:, 1:2], AF.Exp, [rst], [rst], scale=-0.5)
            self.act(self.xs[:, :], xt, AF.Copy, [rx, rst], [self.r_xs], scale=st[:, 1:2])
            for half in range(2):
                bank, rb = self.nb()
                bv = self.bankb(bank)
                for c in range(4):
                    cc = half * 4 + c
                    self.tr(bv[:, c * 128:(c + 1) * 128], self.xs[:, cc * 128:(cc + 1) * 128], [self.r_xs], [rb])
                self.tt(self.hT[:, half * 4:(half + 1) * 4, t * 128:(t + 1) * 128], v3(bv[:, 0:512], 4),
                        self.vecf[:, nwc + half * 4:nwc + half * 4 + 4].unsqueeze(2).to_broadcast([128, 4, 128]),
                        ALU.mult, [rb, self.r_layer], [self.r_hT])

    def proj_fm(self, W, m0, rw, evac):
        bank, rb = self.nb()
        for kc in range(8):
            self.mm(bank[:, :], W[:, kc, m0:m0 + 128], self.hT[:, kc, :], kc == 0, kc == 7, [rw, self.r_hT], [rb])
        evac(bank, rb)

    def proj_tm(self, W, rw, t, evac):
        bank, rb = self.nb()
        for kc in range(8):
            self.mm(bank[:, :], self.hT[:, kc, t * 128:(t + 1) * 128], W[:, kc, 0:512], kc == 0, kc == 7,
                    [rw, self.r_hT], [rb])
        evac(bank, rb)

    def post_norm_residual(self, mo, rmo, wrow, xt, rx):
        st, rst = self.small()
        self.act(self.xs[:, :], mo, AF.Square, [rmo], [self.r_xs, rst], accum_out=st[:, 0:1])
        self.act(st[:, 1:2], st[:, 0:1], AF.Ln, [rst, self.r_cst], [rst], bias=self.cst[:, 1:2], scale=1.0 / D)
        self.act(st[:, 1:2], st[:, 1:2], AF.Exp, [rst], [rst], scale=-0.5)
        self.stt(mo, mo, st[:, 1:2], wrow, ALU.mult, ALU.mult, [rmo, rst, self.r_layer], [rmo])
        self.tt(xt, xt, mo, ALU.add, [rx, rmo], [rx])

    def group(self, li, g):
        S = self.S
        T = self.T
        src = self.xsrc(li)
        dst = self.xdst(li)
        self.rot_set = [0, 1, 2, 3, 4, 5]
        for t in range(4):
            tt_ = g * 4 + t
            S.dma("sp", self.xg[:, t, :], src[tt_ * 128:(tt_ + 1) * 128, :], reads=[self.r_xrow[li][tt_]],
                  writes=[self.r_xg[t]])
        STOP = getattr(self, "stop", 99)
        if STOP >= 1:
            self.group_body(li, g, STOP)
        for t in range(4):
            tt_ = g * 4 + t
            S.dma("sp", dst[tt_ * 128:(tt_ + 1) * 128, :], self.xg[:, t, :], reads=[self.r_xg[t]],
                  writes=[self.r_xrow[li + 1][tt_]])

    def group_body(self, li, g, STOP):
        S = self.S
        self.norm_T(0)
        if STOP < 2:
            return

        wb, rw = self.load_block(li, "B0")
        W = v3(wb[:, 0:4224], 8)
        for m in range(4):
            self.proj_fm(W, m * 128, rw,
                         lambda bank, rb, m=m: self.cp("act", self.qkg[:, m, :], bank[:, :], [rb], [self.r_qkg]))
        if STOP < 2.2:
            return
        bank, rb = self.nb()
        for kc in range(8):
            self.mm(bank[0:16, :], W[:, kc, 512:528], self.hT[:, kc, :], kc == 0, kc == 7, [rw, self.r_hT], [rb])
        self.cp("act", self.g1T[:, :], bank[0:16, :], [rb], [self.r_g1T])
        self.done_block()
        if STOP < 2.4:
            return
        wb, rw = self.load_block(li, "B1")
        W = v3(wb[:, 0:4096], 8)
        for t in range(4):
            self.proj_tm(W, rw, t, lambda bank, rb, t=t: self.cp("act", self.vc[:, t, :], bank[:, :], [rb], [self.r_vc[t]]))
        self.done_block()
        if STOP < 2.6:
            return
        wb, rw = self.load_block(li, "B2")
        W = v3(wb[:, 0:4096], 8)
        for t in range(4):
            self.proj_tm(W, rw, t, lambda bank, rb, t=t: self.act(self.rq[:, t, :], bank[:, :], AF.Silu, [rb], [self.r_rq]))
        self.done_block()
        if STOP < 2.8:
            return
        wb, rw = self.load_block(li, "B3")
        W = v3(wb[:, 0:4096], 8)
        for t in range(4):
            self.proj_tm(W, rw, t, lambda bank, rb, t=t: self.cp("act", self.u[:, 1 + t, :], bank[:, :], [rb], [self.r_u[1 + t]]))
        self.done_block()

        if STOP < 3:
            return
        for t in range(4):
            self.gla_tile(g, t)
        if STOP < 3.99:
            return
        if STOP < 4:
            return
        for t in range(4):
            self.pool_tile(g, t)
        if STOP < 5:
            return
        self.cp("dve", self.u[:, 0, :], self.u[:, 4, :], [self.r_u[4]], [self.r_u[0]])

        wb, rw = self.load_block(li, "B4")
        W = v3(wb[:, 0:4096], 8)
        for m in range(4):
            def evq(bank, rb, m=m):
                self.cp("act", self.rq[:, m, :], bank[:, :], [rb], [self.r_rq])
            self.proj_fm(W, m * 128, rw, evq)
        self.done_block()
        wb, rw = self.load_block(li, "B5")
        W = v3(wb[:, 0:4096], 8)
        for m in range(4):
            def ev(bank, rb, m=m):
                self.cp("act", self.kTm[:, m, g * 512:(g + 1) * 512], bank[:, :], [rb], [self.r_kTm[g]])
                self.red(self.ksum[:, 2 * m:2 * m + 2], v3(bank[:, :], 2), ALU.add, [rb], [self.r_ksum])
            self.proj_fm(W, m * 128, rw, ev)
        self.done_block()
        ks = v3(self.ksum[:, 0:8], 4)
        for (r0, c0) in ((0, 2 * g), (64, 16 + 2 * g)):
            rows = slice(r0, r0 + 64)
            cs = slice(c0, c0 + 2)
            self.ts(self.kmF[rows, :, cs], ks[rows, :, :], 1.0 / 256, ALU.mult, [self.r_ksum], [self.r_kmBD])
            self.cp("dve", self.kmBD[rows, :, cs], self.kmF[rows, :, cs], [self.r_kmBD], [self.r_kmBD])
        wb, rw = self.load_block(li, "B6")
        W = v3(wb[:, 0:4096], 8)
        for t in range(4):
            tt_ = g * 4 + t
            self.proj_tm(W, rw, t, lambda bank, rb, tt_=tt_: self.cp("act", self.Vaug[:, tt_, :, 0:64], v3(bank[:, :], 8),
                                                                      [rb], [self.r_V[tt_]]))
        self.done_block()
        if STOP < 6:
            return
        self.moba_group(g)
        if STOP < 7:
            return
        self.mix_out(li, g)
        if STOP < 8:
            return
        self.ffn(li, g)

    def gla_tile(self, g, t):
        tt_ = g * 4 + t
        cols = slice(t * 128, (t + 1) * 128)
        rl = self.r_layer
        bank, rb = self.nb()
        self.mm(bank[:, 0:256], self.g1T[0:16, cols], self.wg2[0:16, :], True, True, [self.r_g1T, self.r_layerb], [rb])
        lg, rlg = self.t2k()
        self.tt(lg[:, 0:256], bank[:, 0:256], self.rowbc[:, 2048:2304], ALU.add, [rb, rl], [rlg])
        self.act(lg[:, 0:256], lg[:, 0:256], AF.Exp, [rlg], [rlg], scale=-1.0)
        la, rla = self.t2k()
        self.act(la[:, 0:256], lg[:, 0:256], AF.Ln, [rlg, self.r_cst], [rla], bias=self.cst[:, 0:1], scale=1.0)
        if getattr(self, "stop", 99) < 3.1:
            return
        lhi, rlhi = self.t1k()
        self.cp("dve", lhi[:, 0:256], la[:, 0:256], [rla], [rlhi])
        llo, rllo = self.t1k()
        self.tt(llo[:, 0:256], la[:, 0:256], lhi[:, 0:256], ALU.subtract, [rla, rlhi], [rllo])
        cum, rcum = self.nb()
        for p in range(2):
            self.mm(cum[:, p * 128:(p + 1) * 128], lhi[:, p * 128:(p + 1) * 128], self.triB[:, :], True, False,
                    [rlhi, self.r_const], [rcum])
            self.mm(cum[:, p * 128:(p + 1) * 128], llo[:, p * 128:(p + 1) * 128], self.triB[:, :], False, True,
                    [rllo, self.r_const], [rcum])
        if getattr(self, "stop", 99) < 3.12:
            return
        eq, req = self.t2k()
        self.act(eq[:, 0:256], cum[:, 0:256], AF.Exp, [rcum], [req], scale=-1.0 / 16)
        ek, rek = self.t2k()
        self.act(ek[:, 0:256], cum[:, 0:256], AF.Exp, [rcum], [rek], scale=1.0 / 16)
        if getattr(self, "stop", 99) < 3.13:
            return
        st, rst = self.small()
        for p in range(2):
            self.ts(st[:, p:p + 1], cum[:, p * 128 + 127:p * 128 + 128], -1.0 / 16, ALU.mult, [rcum], [rst])
        self.act(st[:, 2:4], st[:, 0:2], AF.Exp, [rst], [rst])
        ekk, rekk = self.t2k()
        for p in range(2):
            self.act(ekk[:, p * 128:(p + 1) * 128], cum[:, p * 128:(p + 1) * 128], AF.Exp, [rcum, rst], [rekk],
                     bias=st[:, p:p + 1], scale=1.0 / 16)
        if getattr(self, "stop", 99) < 3.2:
            return
        qp, rqp = self.t1k()
        self.stt(v3(qp[:, 0:256], 2), self.qkg[:, 0:2, cols], 0.125, v3(eq[:, 0:256], 2), ALU.mult, ALU.mult,
                 [self.r_qkg, req], [rqp])
        kp, rkp = self.t1k()
        self.tt(v3(kp[:, 0:256], 2), self.qkg[:, 2:4, cols], v3(ek[:, 0:256], 2), ALU.mult, [self.r_qkg, rek], [rkp])
        kpp, rkpp = self.t1k()
        self.tt(v3(kpp[:, 0:256], 2), self.qkg[:, 2:4, cols], v3(ekk[:, 0:256], 2), ALU.mult, [self.r_qkg, rekk], [rkpp])
        if getattr(self, "stop", 99) < 3.3:
            return
        bank, rb = self.nb()
        bv = self.bankb(bank)
        for p in range(2):
            self.tr(bv[:, p * 128:(p + 1) * 128], kpp[:, p * 128:(p + 1) * 128], [rkpp], [rb])
        kt, rkt = self.t1k()
        self.cp("dve", kt[:, 0:256], bv[:, 0:256], [rb], [rkt])
        if getattr(self, "stop", 99) < 3.4:
            return
        scs = [self.nb(), self.nb()]
        for h in range(4):
            p, hh = h // 2, h % 2
            hb = hh * 64
            self.mm(scs[hh][0][:, p * 128:(p + 1) * 128], kp[hb:hb + 64, p * 128:(p + 1) * 128],
                    qp[hb:hb + 64, p * 128:(p + 1) * 128], True, True, [rkp, rqp], [scs[hh][1]])
        am, ram = self.t1k()
        amv = am[:, :].rearrange("p (a b c) -> p a b c", a=2, b=2)
        for hh in range(2):
            self.tt(amv[:, :, hh, :], v3(scs[hh][0][:, 0:256], 2), self.triB[:, :].unsqueeze(1).to_broadcast([128, 2, 128]),
                    ALU.mult, [scs[hh][1], self.r_const], [ram])
        if getattr(self, "stop", 99) < 3.5:
            return
        ob, rob = self.nb()
        for h in range(4):
            p, hb = h // 2, (h % 2) * 64
            self.mm(ob[:, h * 128:(h + 1) * 128], am[:, h * 128:(h + 1) * 128], self.vc[:, t, h * 128:(h + 1) * 128],
                    True, tt_ == 0, [ram, self.r_vc[t]], [rob])
            if tt_ > 0:
                self.mm(ob[:, h * 128:(h + 1) * 128], qp[hb:hb + 64, p * 128:(p + 1) * 128],
                        self.Sb[hb:hb + 64, p * 128:(p + 1) * 128], False, True, [rqp, self.r_Sb], [rob])
        if getattr(self, "stop", 99) < 3.6:
            return
        ds, rds = self.nb()
        for p in range(2):
            self.mm(ds[:, p * 256:(p + 1) * 256], kt[:, p * 128:(p + 1) * 128], self.vc[:, t, p * 256:(p + 1) * 256],
                    True, True, [rkt, self.r_vc[t]], [rds])
        for p in range(2):
            for hh in range(2):
                rows = slice(hh * 64, hh * 64 + 64)
                dsv = ds[rows, p * 256 + hh * 128:p * 256 + hh * 128 + 128]
                sfv = self.Sf[rows, p * 128:(p + 1) * 128]
                if tt_ == 0:
                    self.cp("dve", sfv, dsv, [rds], [self.r_Sf])
                else:
                    self.stt(sfv, sfv, st[rows, 2 + p:3 + p], dsv, ALU.mult, ALU.add, [self.r_Sf, rst, rds], [self.r_Sf])
        self.cp("act", self.Sb[:, :], self.Sf[:, :], [self.r_Sf], [self.r_Sb])
        if getattr(self, "stop", 99) < 3.7:
            return
        of, rof = self.t2k()
        self.cp("act", of[:, :], ob[:, :], [rob], [rof])
        sq, rsq = self.t2k()
        self.tt(sq[:, :], of[:, :], of[:, :], ALU.mult, [rof], [rsq])
        st2, rst2 = self.small()
        self.red(st2[:, 0:4], v3(sq[:, :], 4), ALU.add, [rsq], [rst2])
        self.act(st2[:, 4:8], st2[:, 0:4], AF.Ln, [rst2, self.r_cst], [rst2], bias=self.cst[:, 1:2], scale=1.0 / 128)
        self.act(st2[:, 4:8], st2[:, 4:8], AF.Exp, [rst2], [rst2], scale=-0.5)
        self.tt(v3(of[:, :], 4), v3(of[:, :], 4), st2[:, 4:8].unsqueeze(2).to_broadcast([128, 4, 128]), ALU.mult,
                [rof, rst2], [rof])
        self.tt(v3(of[:, :], 4), v3(of[:, :], 4), self.rowbc[:, 2304:2432].unsqueeze(1).to_broadcast([128, 4, 128]),
                ALU.mult, [rof, rl], [rof])
        A, rA = self.t1k()
        self.tt(A[:, :], of[:, :], self.rq[:, t, :], ALU.mult, [rof, self.r_rq], [rA])
        bank, rb = self.nb()
        bv = self.bankb(bank)
        for c in range(4):
            self.tr(bv[:, c * 128:(c + 1) * 128], A[:, c * 128:(c + 1) * 128], [rA], [rb])
        self.cp("dve", self.big[:, 8:12, cols], v3(bv[:, 0:512], 4), [rb], self.r_big[8:12])

    def pool_tile(self, g, t):
        tt_ = g * 4 + t
        cols = slice(t * 128, (t + 1) * 128)
        bank, rb = self.nb()
        for gi in range(4):
            gs = slice(gi * 128, (gi + 1) * 128)
            if tt_ == 0:
                self.mm(bank[:, gs], self.u[:, 1 + t, gs], self.cpool[:, 8 + gi, :], True, True,
                        [self.r_u[1 + t], self.r_const], [rb])
            else:
                self.mm(bank[:, gs], self.u[:, 1 + t, gs], self.cpool[:, gi, :], True, False,
                        [self.r_u[1 + t], self.r_const], [rb])
                self.mm(bank[:, gs], self.u[:, t, gs], self.cpool[:, 4 + gi, :], False, True,
                        [self.r_u[t], self.r_const], [rb])
        pT, rpT = self.t1k()
        self.cp("act", pT[:, :], bank[:, :], [rb], [rpT])
        b2, rb2 = self.nb()
        for gi in range(4):
            gs = slice(gi * 128, (gi + 1) * 128)
            self.mm(b2[:, gs], self.wpool[:, gi, :], pT[:, gs], True, True, [self.r_layerb, rpT], [rb2])
        self.tt(self.big[:, 12:16, cols], v3(b2[:, :], 4), self.vecf[:, 16:20].unsqueeze(2).to_broadcast([128, 4, 128]),
                ALU.mult, [rb2, self.r_layer], self.r_big[12:16])

    def moba_group(self, g):
        qT = self.rq
        rq = self.r_rq
        self.memset("dve", self.selb[:, :, :], 0.0, [self.r_selb])
        for t in range(4):
            tt_ = g * 4 + t
            bt = tt_ // 2
            if bt < 4:
                continue
            cols = slice(t * 128, (t + 1) * 128)
            bank, rb = self.nb()
            for p in range(4):
                ps_ = bank[:, p * 32:(p + 1) * 32]
                self.mm(ps_, qT[:, p, cols], self.kmBD[:, p, :], True, True, [rq, self.r_kmBD], [rb])
            Ga, rGa = self.t2k()
            Gb, rGb = self.t2k()
            Gc, rGc = self.t2k()
            Gd, rGd = self.t2k()
            self.cp("dve", Ga[:, 0:128], bank[:, 0:128], [rb], [rGa])

            def V(x):
                return v3(x[:, 0:128], 8)[:, :, 0:bt]
            m, rm = self.small()

            def bc(col):
                return col.unsqueeze(2).to_broadcast([128, 8, bt])
            self.red(m[:, 0:8], V(Ga), ALU.max, [rGa], [rm])
            self.tt(V(Gb), V(Ga), bc(m[:, 0:8]), ALU.is_ge, [rGa, rm], [rGb])
            self.stt(V(Gc), V(Gb), NEGBIG, V(Ga), ALU.mult, ALU.add, [rGb, rGa], [rGc])
            m2, rm2 = self.small()
            self.red(m2[:, 0:8], V(Gc), ALU.max, [rGc], [rm2])
            self.tt(V(Gb), V(Gc), bc(m2[:, 0:8]), ALU.is_ge, [rGc, rm2], [rGb])
            self.stt(V(Gd), V(Gb), NEGBIG, V(Gc), ALU.mult, ALU.add, [rGb, rGc], [rGd])
            m3, rm3 = self.small()
            self.red(m3[:, 0:8], V(Gd), ALU.max, [rGd], [rm3])
            self.tt(V(Gb), V(Ga), bc(m3[:, 0:8]), ALU.is_lt, [rGa, rm3], [rGb])
            self.ts(v3(self.selb[:, t, :], 8)[:, :, 0:bt], V(Gb), NEGV, ALU.mult, [rGb], [self.r_selb])

        if self.dbg and self.cur_li == 0:
            self.S.dma("sp", self.dbg_selb[g], self.selb[:, :, :], reads=[self.r_selb], writes=[self.r_dbg])
        for h in range(8):
            p, hb = h // 2, (h % 2) * 64
            bank, rb = self.nb()
            bv = self.bankb(bank)
            for t in range(4):
                self.tr(bv[hb:hb + 16, t * 128:(t + 1) * 128], self.selb[:, t, h * 16:(h + 1) * 16], [self.r_selb], [rb])
            selT, rselT = self.selTb[h % 2], self.r_selTb[h % 2]
            self.cp("dve", selT[hb:hb + 16, :], bv[hb:hb + 16, 0:512], [rb], [rselT])
            if self.dbg and self.cur_li == 0 and g == 2 and h == 2:
                self.S.dma("sp", self.dbg_selT[:, :], selT[hb:hb + 16, :], reads=[rselT], writes=[self.r_dbg3])
            oi = 6 + (h % 2)
            ob, rob = self.bank[oi], self.r_bank[oi]
            self.memset("dve", ob[:, 0:260], 0.0, [rob])
            for i in range(4 * g + 4):
                n = i // 2
                js = [j for j in range(4) if 4 * g + j >= i and 4 * g + j - i <= WIN[h]]
                if not js:
                    continue
                own = [j for j in js if (4 * g + j) // 2 == n]
                past = [j for j in js if (4 * g + j) // 2 > n]
                sb_, rsb = self.nb()
                kap = self.kTm[hb:hb + 64, p, i * 128:(i + 1) * 128]
                rk = self.r_kTm[i // 4]
                if past:
                    pc = slice(past[0] * 128, (past[-1] + 1) * 128)
                    self.mm(sb_[:, pc], self.e16[hb:hb + 16, n, :], selT[hb:hb + 16, pc], True, False,
                            [self.r_const, rselT], [rsb])
                    self.mm(sb_[:, pc], kap, qT[hb:hb + 64, p, pc], False, True, [rk, rq], [rsb])
                if own:
                    jj = list(own)
                    if 4 * g + jj[0] == i:
                        dc = slice(jj[0] * 128, (jj[0] + 1) * 128)
                        self.mm(sb_[:, dc], kap, qT[hb:hb + 64, p, dc], True, False, [rk, rq], [rsb])
                        self.mm(sb_[:, dc], self.identB[:, :], self.negtri[:, :], False, True, [self.r_const], [rsb])
                        jj = jj[1:]
                    if jj:
                        oc = slice(jj[0] * 128, (jj[-1] + 1) * 128)
                        self.mm(sb_[:, oc], kap, qT[hb:hb + 64, p, oc], True, True, [rk, rq], [rsb])
                PT, rPT = self.t1k()
                if h < 2:
                    for j in js:
                        d = 4 * g + j + 1 - i
                        jc = slice(j * 128, (j + 1) * 128)
                        self.act(PT[:, jc], sb_[:, jc], AF.Exp, [rsb, self.r_constf], [rPT],
                                 bias=self.alibi[:, h * 33 + d:h * 33 + d + 1], scale=0.125)
                else:
                    d = 4 * g + 4 - i
                    ac = slice(js[0] * 128, (js[-1] + 1) * 128)
                    self.act(PT[:, ac], sb_[:, ac], AF.Exp, [rsb, self.r_constf], [rPT],
                             bias=self.alibi[:, h * 33 + d:h * 33 + d + 1], scale=0.125)
                if self.dbg and self.cur_li == 0 and g == 2 and h == 2 and i == 7:
                    self.S.dma("sp", self.dbg_PT[:, :], PT[:, :], reads=[rPT], writes=[self.r_dbg4])
                for j in js:
                    jt = 4 * g + j
                    first_i = max(0, jt - WIN[h])
                    self.mm(ob[:, j * 65:(j + 1) * 65], PT[:, j * 128:(j + 1) * 128], self.Vaug[:, i, h, :],
                            False, False, [rPT, self.r_V[i]], [rob], skip=True)
            rinv, rri = self.small()
            obv = v3(ob[:, 0:260], 4)
            self.S.op("dve", lambda e, o=rinv[:, 0:4], i=obv[:, :, 64]: e.reciprocal(out=o, in_=i), [rob], [rri])
            self.tt(self.vc[:, :, h * 64:(h + 1) * 64], obv[:, :, 0:64], rinv[:, 0:4].unsqueeze(2).to_broadcast([128, 4, 64]),
                    ALU.mult, [rob, rri], self.r_vc)
        if self.dbg and self.cur_li == 0:
            self.S.dma("sp", self.dbg_C[g], self.vc[:, :, :], reads=self.r_vc, writes=[self.r_dbg2])
        for j in range(4):
            bank, rb = self.nb()
            bv = self.bankb(bank)
            for c in range(4):
                self.tr(bv[:, c * 128:(c + 1) * 128], self.vc[:, j, c * 128:(c + 1) * 128], [self.r_vc[j]], [rb])
            self.cp("dve", self.big[:, 16:20, j * 128:(j + 1) * 128], v3(bv[:, 0:512], 4), [rb], self.r_big[16:20])

    def mix_out(self, li, g):
        for c in range(8):
            wb, rw = self.load_block(li, "M%d" % c)
            Wg = wb[:, 0:3072].rearrange("p (k i c) -> p k i c", k=8, i=3)
            Wb = wb[:, 3072:4608].rearrange("p (i k c) -> p i k c", i=3, k=4)
            acc, racc = self.t2k()
            for i in range(3):
                bg, rbg = self.nb()
                for kc in range(8):
                    self.mm(bg[:, :], Wg[:, kc, i, :], self.hT[:, kc, :], kc == 0, kc == 7, [rw, self.r_hT], [rbg])
                sg, rsg = self.t2k()
                self.act(sg[:, :], bg[:, :], AF.Sigmoid, [rbg], [rsg])
                by, rby = self.nb()
                for kc in range(4):
                    ch = 8 + 4 * i + kc
                    self.mm(by[:, :], Wb[:, i, kc, :], self.big[:, ch, :], kc == 0, kc == 3, [rw, self.r_big[ch]], [rby])
                if i == 0:
                    self.tt(acc[:, :], by[:, :], sg[:, :], ALU.mult, [rby, rsg], [racc])
                else:
                    self.tt(sg[:, :], by[:, :], sg[:, :], ALU.mult, [rby, rsg], [rsg])
                    if i == 1:
                        self.tt(acc[:, :], acc[:, :], sg[:, :], ALU.add, [racc, rsg], [racc])
                    else:
                        self.tt(self.big[:, c, :], acc[:, :], sg[:, :], ALU.add, [racc, rsg], [self.r_big[c]])
            self.done_block()
        wo = [self.load_block(li, "O%d" % hf) for hf in range(2)]
        mo = self.qkg[:, 0:2, :].rearrange("p a b -> p (a b)")
        rmo = self.r_qkg
        for t in range(4):
            for hf in range(2):
                W = v3(wo[hf][0][:, 0:4096], 8)
                bank, rb = self.nb()
                for kc in range(8):
                    self.mm(bank[:, :], self.big[:, kc, t * 128:(t + 1) * 128], W[:, kc, :], kc == 0, kc == 7,
                            [wo[hf][1], self.r_big[kc]], [rb])
                self.cp("act", mo[:, hf * 512:(hf + 1) * 512], bank[:, :], [rb], [rmo])
            self.post_norm_residual(mo, rmo, self.rowbc[:, 0:1024], self.xg[:, t, :], self.r_xg[t])
        self.done_block()
        self.done_block()

    def ffn(self, li, g):
        self.norm_T(8)
        self.rot_set = [0, 1, 2, 3]
        for j in range(11):
            wb, rw = self.load_block(li, "GU%d" % j)
            W = wb[:, 0:4096].rearrange("p (k g c) -> p k g c", k=8, g=2)
            for m in range(2):
                bg, rbg = self.nb()
                for kc in range(8):
                    self.mm(bg[:, :], W[:, kc, 0, m * 128:(m + 1) * 128], self.hT[:, kc, :], kc == 0, kc == 7,
                            [rw, self.r_hT], [rbg])
                bu, rbu = self.nb()
                for kc in range(8):
                    self.mm(bu[:, :], W[:, kc, 1, m * 128:(m + 1) * 128], self.hT[:, kc, :], kc == 0, kc == 7,
                            [rw, self.r_hT], [rbu])
                sg, rsg = self.t2k()
                self.act(sg[:, :], bg[:, :], AF.Silu, [rbg], [rsg])
                ch = 2 * j + m
                self.tt(self.big[:, ch, :], bu[:, :], sg[:, :], ALU.mult, [rbu, rsg], [self.r_big[ch]])
            self.done_block()
        mo = self.qkg[:, 0:2, :].rearrange("p a b -> p (a b)")
        rmo = self.r_qkg
        for ps in range(2):
            for j in range(11):
                wb, rw = self.load_block(li, "DN%d" % j)
                Wd = v3(wb[:, 0:2048], 2)
                for m in range(2):
                    ch = 2 * j + m
                    for tl in range(2):
                        t = 2 * ps + tl
                        for hf in range(2):
                            bi = 4 + tl * 2 + hf
                            self.mm(self.bank[bi][:, :], self.big[:, ch, t * 128:(t + 1) * 128],
                                    Wd[:, m, hf * 512:(hf + 1) * 512], ch == 0, ch == 21, [self.r_big[ch], rw],
                                    [self.r_bank[bi]])
                self.done_block()
            for tl in range(2):
                t = 2 * ps + tl
                for hf in range(2):
                    bi = 4 + tl * 2 + hf
                    self.cp("act", mo[:, hf * 512:(hf + 1) * 512], self.bank[bi][:, :], [self.r_bank[bi]], [rmo])
                self.post_norm_residual(mo, rmo, self.rowbc[:, 1024:2048], self.xg[:, t, :], self.r_xg[t])
        self.rot_set = [0, 1, 2, 3, 4, 5]


def make_consts():
    s = np.arange(128)[:, None]
    t = np.arange(128)[None, :]
    c = {}
    c["c_ident"] = np.eye(128, dtype=np.float32)
    c["c_tri"] = (s <= t).astype(np.float32)
    c["c_negtri"] = np.where(s <= t, 0.0, NEGV).astype(np.float32)
    pm = np.zeros((12, 128, 128), np.float32)
    for gi, w in enumerate((2, 4, 8, 16)):
        dlt = t - s
        pm[gi] = np.where((dlt >= 0) & (dlt < w), 1.0 / w, 0.0) - (s == t)
        dprev = t + 128 - s
        pm[4 + gi] = np.where(dprev < w, 1.0 / w, 0.0)
        cnt = np.minimum(t + 1, w).astype(np.float32)
        pm[8 + gi] = np.where((dlt >= 0) & (dlt < w), 1.0 / cnt, 0.0) - (s == t)
    c["c_pool"] = pm
    e = np.zeros((128, 16, 128), np.float32)
    for n in range(16):
        e[n, n, :] = 1.0
        e[64 + n, n, :] = 1.0
    c["c_e16"] = e.reshape(128, 16 * 128)
    al = np.zeros((128, 8, 33), np.float32)
    pp = np.arange(128, dtype=np.float64)[:, None]
    dd = np.arange(33, dtype=np.float64)[None, :]
    for h in range(8):
        al[:, h, :] = SLOPES[h] * (pp + 1.0 - 128.0 * dd)
    c["c_alibi"] = al.reshape(128, 8 * 33)
    return c


def layer_inputs(inp, ls):
    ls = list(ls)
    d = {}
    for k in ("w_in", "gla_w_g2", "pool_w", "w_branch_a", "w_branch_b", "w_branch_c", "w_out",
              "ffn_w_gate", "ffn_w_up", "ffn_w_down"):
        d[k] = np.ascontiguousarray(np.asarray(inp[k], dtype=np.float32)[ls])
    vecf = np.zeros((len(ls), 128, 20), np.float32)
    rowbc = np.zeros((len(ls), 128, 2432), np.float32)
    for i, l in enumerate(ls):
        vecf[i, :, 0:8] = np.asarray(inp["norm_mix_pre"][l]).reshape(8, 128).T
        vecf[i, :, 8:16] = np.asarray(inp["norm_ffn_pre"][l]).reshape(8, 128).T
        vecf[i, :, 16:20] = np.asarray(inp["pool_scale"][l]).reshape(4, 128).T
        rowbc[i, :, 0:1024] = np.asarray(inp["norm_mix_post"][l])[None, :]
        rowbc[i, :, 1024:2048] = np.asarray(inp["norm_ffn_post"][l])[None, :]
        rowbc[i, :, 2048:2304] = np.asarray(inp["gla_b_g"][l])[None, :]
        rowbc[i, :, 2304:2432] = np.asarray(inp["gla_norm"][l])[None, :]
    d["vecf"] = vecf
    d["rowbc"] = rowbc
    d.update(make_consts())
    return d


_PROG_CACHE = {}


STOP_STAGE = 99


def get_prog(T, nl):
    key = (T, nl)
    if key not in _PROG_CACHE:
        p = Prog(T, nl)
        p.stop = STOP_STAGE
        _PROG_CACHE[key] = p.build()
    return _PROG_CACHE[key]


def run_layers(xs, inp, ls):
    T = xs[0].shape[0]
    nc = get_prog(T, len(ls))
    shared = layer_inputs(inp, ls)
    in_maps = []
    for x in xs:
        m = dict(shared)
        m["x"] = np.ascontiguousarray(x, dtype=np.float32)
        in_maps.append(m)
    res = run_bass_kernel_spmd(nc, in_maps, core_ids=list(range(len(xs))))
    return [np.asarray(r["y"]) for r in res.results]


FUSED = True


def kernel(**inputs):
    x = np.asarray(inputs["x"], dtype=np.float32)
    B = x.shape[0]
    xs = [x[b] for b in range(B)]
    if FUSED:
        ys = run_layers(xs, inputs, [0, 1])
    else:
        ys = run_layers(xs, inputs, [0])
        ys = run_layers(ys, inputs, [1])
    return np.stack(ys, axis=0).astype(np.float32)
```
